# Optimizing a Trainium2 kernel written in Bass

```python
import math
import jax, jax.numpy as jnp
from jax import lax
import numpy as np

D_MODEL = 2048
BATCH = 1
SEQ = 8192
DEPTH = 1

HEAD_DIM = 128
A_GROUPS = ((128, 1), (512, 4), (2048, 16))
A_HEADS_PER_GROUP = 4
A_HEADS = A_HEADS_PER_GROUP * len(A_GROUPS)
A_WIDTH = A_HEADS * HEAD_DIM
A_OUT_WIDTH = A_HEADS_PER_GROUP * HEAD_DIM
A_BLOCK = 64
B_Q_HEADS = 8
B_KV_HEADS = 2
B_Q_WIDTH = B_Q_HEADS * HEAD_DIM
B_KV_WIDTH = B_KV_HEADS * HEAD_DIM
B_Q_BLOCK = 128
ROPE_THETA = 10000.0
GRID_W = 64
QK_NORM_EPS = 1e-6
REL_BUCKETS = 32
REL_MAX_DIST = 1024
A_V_OFF = 2 * A_WIDTH
B_Q_OFF = 3 * A_WIDTH
B_V_OFF = B_Q_OFF + B_Q_WIDTH + B_KV_WIDTH
GATE_OFF = B_V_OFF + B_KV_WIDTH
IN_WIDTH = GATE_OFF + 2 * D_MODEL
N_EXPERTS = 32
TOP_K = 4
D_FF = D_MODEL
SWIGLU_LIMIT = 7.0
SWIGLU_ALPHA = 1.702
MOE_BLOCK = 128
LN_EPS = 1e-5

kernel_name = 'hybrid_dilated_swa_axial_gqa_moe_deepnorm'


def _layer_norm(x, g, b):
    xf = x.astype(jnp.float32)
    mu = jnp.mean(xf, axis=-1, keepdims=True)
    var = jnp.mean(jnp.square(xf - mu), axis=-1, keepdims=True)
    return ((xf - mu) * lax.rsqrt(var + LN_EPS) * g + b).astype(x.dtype)


def _rms_heads(t, g):
    tf = t.astype(jnp.float32)
    return (tf * lax.rsqrt(jnp.mean(tf * tf, axis=-1, keepdims=True) + QK_NORM_EPS) * g).astype(t.dtype)


def _t5_bucket(rel):
    nb = REL_BUCKETS // 2
    max_exact = nb // 2
    n = jnp.abs(rel)
    nf = jnp.maximum(n, 1).astype(jnp.float32)
    large = max_exact + (jnp.log(nf / max_exact) / math.log(REL_MAX_DIST / max_exact)
                         * (nb - max_exact)).astype(jnp.int32)
    large = jnp.minimum(large, nb - 1)
    return jnp.where(rel > 0, nb, 0) + jnp.where(n < max_exact, n, large)


def _dilated_group(q, k, v, bias_tab, dilation, half):
    b, s, h, hd = q.shape
    n = s // dilation

    def by_stride(t):
        return t.reshape(b, n, dilation, h, hd).transpose(0, 2, 1, 3, 4).reshape(b * dilation, n, h, hd)

    qs, ks, vs = by_stride(q), by_stride(k), by_stride(v)
    blk = math.gcd(n, A_BLOCK)
    nblk = n // blk
    span = blk + 2 * half
    pad = ((0, 0), (half, half), (0, 0), (0, 0))
    kidx = jnp.arange(nblk)[:, None] * blk + jnp.arange(span)[None, :]
    kb = jnp.pad(ks, pad)[:, kidx]
    vb = jnp.pad(vs, pad)[:, kidx]
    qb = qs.reshape(b * dilation, nblk, blk, h, hd)
    rel = jnp.arange(span)[None, :] - half - jnp.arange(blk)[:, None]
    bias = bias_tab[_t5_bucket(rel * dilation)].transpose(2, 0, 1).astype(jnp.float32)
    kpos = kidx - half
    valid = ((jnp.abs(rel)[None] <= half)
             & (kpos[:, None, :] >= 0) & (kpos[:, None, :] < n))
    logits = (jnp.einsum('znqhd,znkhd->znhqk', qb, kb).astype(jnp.float32) / math.sqrt(hd)
              + bias[None, None])
    logits = jnp.where(valid[None, :, None], logits, -jnp.inf)
    lse = jax.nn.logsumexp(logits, axis=-1)
    p = jnp.exp(logits - lse[..., None]).astype(v.dtype)
    out = jnp.einsum('znhqk,znkhd->znqhd', p, vb)
    out = out.reshape(b, dilation, n, h, hd).transpose(0, 2, 1, 3, 4).reshape(b, s, h, hd)
    lse = lse.transpose(0, 1, 3, 2).reshape(b, dilation, n, h).transpose(0, 2, 1, 3).reshape(b, s, h)
    return out, lse


def _dilated_mixer(qa, ka, va, rel_bias):
    b, s = qa.shape[:2]
    outs, lses = [], []
    for g, (window, dil) in enumerate(A_GROUPS):
        sl = slice(g * A_HEADS_PER_GROUP, (g + 1) * A_HEADS_PER_GROUP)
        o, l = _dilated_group(qa[:, :, sl], ka[:, :, sl], va[:, :, sl], rel_bias[:, sl], dil, window // (2 * dil))
        outs.append(o)
        lses.append(l)
    w = jax.nn.softmax(jnp.stack(lses, axis=0), axis=0)
    o = jnp.sum(w[..., None].astype(qa.dtype) * jnp.stack(outs, axis=0), axis=0)
    return o.reshape(b, s, A_OUT_WIDTH)


def _rope_axis(t, pos):
    dim = t.shape[-1]
    inv = ROPE_THETA ** (-jnp.arange(0, dim, 2, dtype=jnp.float32) / dim)
    ang = pos.astype(jnp.float32)[:, None] * inv[None, :]
    cos = jnp.cos(ang)[None, :, None, :]
    sin = jnp.sin(ang)[None, :, None, :]
    t1, t2 = jnp.split(t.astype(jnp.float32), 2, axis=-1)
    return jnp.concatenate([t1 * cos - t2 * sin, t1 * sin + t2 * cos], axis=-1).astype(t.dtype)


def _axial_rope(t, row_pos, col_pos):
    half = t.shape[-1] // 2
    return jnp.concatenate([_rope_axis(t[..., :half], row_pos), _rope_axis(t[..., half:], col_pos)], axis=-1)


def _gqa_blocks(q, k, v):
    b, s, hq, hd = q.shape
    hkv = k.shape[2]
    grp = hq // hkv
    nqb = s // B_Q_BLOCK
    qb = q.reshape(b, nqb, B_Q_BLOCK, hkv, grp, hd).transpose(1, 0, 2, 3, 4, 5)
    scale = 1.0 / math.sqrt(hd)

    def one_block(qblk):
        logits = jnp.einsum('bqkgd,bskd->bkgqs', qblk, k).astype(jnp.float32) * scale
        p = jax.nn.softmax(logits, axis=-1).astype(v.dtype)
        return jnp.einsum('bkgqs,bskd->bqkgd', p, v)

    out = lax.map(one_block, qb)
    return out.transpose(1, 0, 2, 3, 4, 5).reshape(b, s, hq * hd)


def _hybrid_mixer(x, w_in, b_gates, rel_bias, q_norm, k_norm, w_branch_a, w_branch_b, w_out, row_pos, col_pos):
    b, s, _ = x.shape
    proj = x @ w_in
    cuts = [A_WIDTH, 2 * A_WIDTH, B_Q_OFF, B_Q_OFF + B_Q_WIDTH, B_V_OFF, GATE_OFF]
    qa, ka, va, qb, kb, vb, gates = jnp.split(proj, cuts, axis=-1)
    heads = lambda t, h: t.reshape(b, s, h, HEAD_DIM)
    ya = _dilated_mixer(heads(qa, A_HEADS), heads(ka, A_HEADS), heads(va, A_HEADS), rel_bias)
    qb = _axial_rope(_rms_heads(heads(qb, B_Q_HEADS), q_norm), row_pos, col_pos)
    kb = _axial_rope(_rms_heads(heads(kb, B_KV_HEADS), k_norm), row_pos, col_pos)
    yb = _gqa_blocks(qb, kb, heads(vb, B_KV_HEADS))
    g_a, g_b = jnp.split(jax.nn.sigmoid(gates + b_gates), 2, axis=-1)
    y = g_a * (ya @ w_branch_a) + g_b * (yb @ w_branch_b)
    return y @ w_out


def _moe(x, w_router, b_router, w_gate, b_gate, w_lin, b_lin, w_down, b_down):
    b, s, d = x.shape
    t = b * s
    xf = x.reshape(t, d)
    logits = (xf @ w_router).astype(jnp.float32) + b_router.astype(jnp.float32)
    top_vals, top_idx = lax.top_k(logits, TOP_K)
    gate_w = jax.nn.softmax(top_vals, axis=-1)
    a = t * TOP_K
    flat_e = top_idx.reshape(a)
    order = jnp.argsort(flat_e)
    e_sorted = flat_e[order]
    tok_sorted = (order // TOP_K).astype(jnp.int32)
    w_sorted = gate_w.reshape(a)[order]
    sizes = jnp.bincount(flat_e, length=N_EXPERTS)
    padded = (sizes + MOE_BLOCK - 1) // MOE_BLOCK * MOE_BLOCK
    starts = jnp.cumsum(sizes) - sizes
    pends = jnp.cumsum(padded)
    pstarts = pends - padded
    dest = pstarts[e_sorted] + jnp.arange(a) - starts[e_sorted]
    p_slots = a + N_EXPERTS * MOE_BLOCK
    nblk = p_slots // MOE_BLOCK
    slot_tok = jnp.full((p_slots,), t, jnp.int32).at[dest].set(tok_sorted)
    slot_w = jnp.zeros((p_slots,), jnp.float32).at[dest].set(w_sorted)
    blk_e = jnp.clip(jnp.searchsorted(pends, jnp.arange(nblk) * MOE_BLOCK, side='right'), 0, N_EXPERTS - 1)
    xs = xf[jnp.minimum(slot_tok, t - 1)].reshape(nblk, MOE_BLOCK, d)

    def expert_block(args):
        xb, e = args
        g = jnp.minimum(xb @ w_gate[e] + b_gate[e], SWIGLU_LIMIT)
        lin = jnp.clip(xb @ w_lin[e] + b_lin[e], -SWIGLU_LIMIT, SWIGLU_LIMIT)
        h = (lin + 1.0) * (g * jax.nn.sigmoid(SWIGLU_ALPHA * g))
        return h @ w_down[e] + b_down[e]

    ys = lax.map(expert_block, (xs, blk_e)).reshape(p_slots, d)
    ys = ys * slot_w[:, None].astype(ys.dtype)
    out = jax.ops.segment_sum(ys, slot_tok, num_segments=t + 1)[:t]
    return out.reshape(b, s, d)


def setup_inputs(seed: int = 0) -> dict:
    key = jax.random.key(seed)
    ks = jax.random.split(key, 24)
    beta = (8.0 * DEPTH) ** -0.25
    f32 = jnp.float32

    def nrm(k, shape, fan_in, scale=1.0):
        return jax.random.normal(k, shape, f32) * (scale * fan_in ** -0.5)

    def small(k, shape, scale):
        return jax.random.normal(k, shape, f32) * scale

    col_scale = (jnp.ones((IN_WIDTH,), f32)
                 .at[A_V_OFF:A_V_OFF + A_WIDTH].set(beta)
                 .at[B_V_OFF:B_V_OFF + B_KV_WIDTH].set(beta))
    return {
        'x': jax.random.normal(ks[0], (BATCH, SEQ, D_MODEL), f32),
        'w_in': nrm(ks[1], (DEPTH, D_MODEL, IN_WIDTH), D_MODEL) * col_scale,
        'b_gates': small(ks[2], (DEPTH, 2 * D_MODEL), 0.1),
        'rel_bias': small(ks[3], (REL_BUCKETS, A_HEADS), 0.5),
        'q_norm': 1.0 + small(ks[4], (DEPTH, HEAD_DIM), 0.02),
        'k_norm': 1.0 + small(ks[5], (DEPTH, HEAD_DIM), 0.02),
        'w_branch_a': nrm(ks[6], (DEPTH, A_OUT_WIDTH, D_MODEL), A_OUT_WIDTH, beta),
        'w_branch_b': nrm(ks[7], (DEPTH, B_Q_WIDTH, D_MODEL), B_Q_WIDTH, beta),
        'w_out': nrm(ks[8], (DEPTH, D_MODEL, D_MODEL), D_MODEL, beta),
        'ln1_g': 1.0 + small(ks[9], (DEPTH, D_MODEL), 0.02),
        'ln1_b': small(ks[10], (DEPTH, D_MODEL), 0.02),
        'w_router': nrm(ks[11], (DEPTH, D_MODEL, N_EXPERTS), D_MODEL),
        'b_router': small(ks[12], (DEPTH, N_EXPERTS), 0.01),
        'w_gate': nrm(ks[13], (DEPTH, N_EXPERTS, D_MODEL, D_FF), D_MODEL),
        'b_gate': small(ks[14], (DEPTH, N_EXPERTS, D_FF), 0.02),
        'w_lin': nrm(ks[15], (DEPTH, N_EXPERTS, D_MODEL, D_FF), D_MODEL),
        'b_lin': small(ks[16], (DEPTH, N_EXPERTS, D_FF), 0.02),
        'w_down': nrm(ks[17], (DEPTH, N_EXPERTS, D_FF, D_MODEL), D_FF, beta),
        'b_down': small(ks[18], (DEPTH, N_EXPERTS, D_MODEL), 0.02),
        'ln2_g': 1.0 + small(ks[19], (DEPTH, D_MODEL), 0.02),
        'ln2_b': small(ks[20], (DEPTH, D_MODEL), 0.02),
    }


def reference(x, w_in, b_gates, rel_bias, q_norm, k_norm, w_branch_a, w_branch_b, w_out, ln1_g, ln1_b,
              w_router, b_router, w_gate, b_gate, w_lin, b_lin, w_down, b_down, ln2_g, ln2_b):
    alpha = (2.0 * DEPTH) ** 0.25
    s = x.shape[1]
    rows = s // GRID_W
    row_pos = jnp.repeat(jnp.arange(rows, dtype=jnp.int32), GRID_W)
    col_pos = jnp.tile(jnp.arange(GRID_W, dtype=jnp.int32), rows)
    for l in range(DEPTH):
        mix = _hybrid_mixer(x, w_in[l], b_gates[l], rel_bias, q_norm[l], k_norm[l],
                            w_branch_a[l], w_branch_b[l], w_out[l], row_pos, col_pos)
        x = _layer_norm(alpha * x + mix, ln1_g[l], ln1_b[l])
        ffn = _moe(x, w_router[l], b_router[l], w_gate[l], b_gate[l], w_lin[l], b_lin[l], w_down[l], b_down[l])
        x = _layer_norm(alpha * x + ffn, ln2_g[l], ln2_b[l])
    return x
```

```python
import contextlib
import math
import numpy as np
import concourse.bass as bass
import concourse.mybir as mybir
from concourse.bass_utils import run_bass_kernel_spmd

F32 = mybir.dt.float32
BF16 = mybir.dt.bfloat16
AF = mybir.ActivationFunctionType
ALU = mybir.AluOpType

S = 8192
D = 2048
HD = 128
IN_W = 10240
NE = 32
TB = 512
NTB = S // TB
A_GROUPS = ((128, 1), (512, 4), (2048, 16))
ALPHA = 2.0 ** 0.25
LN_EPS = 1e-5
QK_EPS = 1e-6
NEG = -30000.0


class Res:
    __slots__ = ("name", "w", "rd")

    def __init__(self, name):
        self.name = name
        self.w = None
        self.rd = {}


class Ev:
    __slots__ = ("kind", "eng", "seq", "marked", "sem", "val")

    def __init__(self, kind, eng, seq):
        self.kind = kind
        self.eng = eng
        self.seq = seq
        self.marked = False
        self.sem = None
        self.val = None


class Ins:
    __slots__ = ("fn", "waits", "ev")


ENGS = ("pe", "act", "dve", "pool", "sp")
EPOCH = 30000


class Sched:
    def __init__(self):
        self.streams = {e: [] for e in ENGS}
        self.waited = {e: {} for e in ENGS}
        self.dcount = {}

    def _key(self, ev):
        return ev.eng

    def _add(self, eng, fn, reads, writes, ev):
        ins = Ins()
        ins.fn = fn
        ins.ev = ev
        ins.waits = []
        deps = []
        for r in reads:
            if r.w is not None:
                deps.append(r.w)
        for w in writes:
            if w.w is not None:
                deps.append(w.w)
            deps.extend(w.rd.values())
        wd = self.waited[eng]
        for d in deps:
            if d is ev:
                continue
            if d.kind == "c" and d.eng == "pe" and eng == "pe":
                continue
            k = d.eng
            if wd.get(k, -1) >= d.seq:
                continue
            wd[k] = d.seq
            d.marked = True
            ins.waits.append(d)
        for r in reads:
            r.rd[ev.eng] = ev
        for w in writes:
            w.w = ev
            w.rd = {}
        self.streams[eng].append(ins)
        return ins

    def op(self, eng, fn, reads=(), writes=()):
        ev = Ev("c", eng, len(self.streams[eng]))
        self._add(eng, fn, reads, writes, ev)

    def dma(self, queue, fn, dsem, reads=(), writes=()):
        c = self.dcount.get(dsem, 0) + 1
        self.dcount[dsem] = c
        ev = Ev("d", dsem, c)
        ev.marked = True
        self._add(queue, fn, reads, writes, ev)
        return ev

    def emit(self, nc, final_events, name=""):
        sems = nc._k_sem_stack
        cst = nc._k_csem
        dst = nc._k_dsem
        with contextlib.ExitStack() as st:
            for e in ENGS:
                for ins in self.streams[e]:
                    ev = ins.ev
                    if ev.kind == "c" and ev.marked:
                        cur = cst.get(e)
                        if cur is None or cur[1] >= EPOCH:
                            nsem = sems.enter_context(nc.semaphore(f"c_{e}_{len(sems._exit_callbacks)}"))
                            cur = [nsem, 0]
                            cst[e] = cur
                        cur[1] += 1
                        ev.sem = cur[0]
                        ev.val = cur[1]
            base = {}
            for k in self.dcount:
                if k not in dst:
                    dst[k] = [sems.enter_context(nc.semaphore(f"d_{k}")), 0]
                base[k] = dst[k][1]
            for e in ENGS:
                for ins in self.streams[e]:
                    if ins.ev.kind == "d":
                        ins.ev.sem = dst[ins.ev.eng][0]
                        ins.ev.val = 16 * (base[ins.ev.eng] + ins.ev.seq)
            for k, c in self.dcount.items():
                dst[k][1] = base[k] + c
            block = st.enter_context(nc.Block())

            def run(e):
                def body(h):
                    for ins in self.streams[e]:
                        for d in ins.waits:
                            h.wait_ge(d.sem, d.val)
                        bi = ins.fn(h)
                        ev = ins.ev
                        if ev.kind == "d":
                            bi.then_inc(ev.sem, 16)
                        elif ev.marked:
                            bi.then_inc(ev.sem, 1)
                    for d in final_events:
                        h.wait_ge(d.sem, d.val)
                return body

            block.tensor(run("pe"))
            block.scalar(run("act"))
            block.vector(run("dve"))
            block.gpsimd(run("pool"))
            block.sync(run("sp"))


def _t5_bucket(rel):
    nb = 16
    max_exact = 8
    n = np.abs(rel)
    nf = np.maximum(n, 1).astype(np.float32)
    large = max_exact + (np.log(nf / max_exact) / math.log(1024 / max_exact) * (nb - max_exact)).astype(np.int32)
    large = np.minimum(large, nb - 1)
    return np.where(rel > 0, nb, 0) + np.where(n < max_exact, n, large)


def _host_consts(rev=False):
    c = {}
    c["ident"] = np.eye(128, dtype=np.float32)
    c["ones"] = np.ones((128, 128), np.float32)
    pt = np.zeros((128, 128), np.float32)
    for base in (0, 64):
        for i in range(32):
            pt[base + i + 32, base + i] = -1.0
            pt[base + i, base + i + 32] = 1.0
    c["ropeP"] = pt
    t = np.arange(S)
    if rev:
        t = t[::-1]
    row = (t // 64).astype(np.float32)
    col = (t % 64).astype(np.float32)
    inv = (10000.0 ** (-np.arange(0, 64, 2, dtype=np.float32) / 64)).astype(np.float32)
    ang_r = row[None, :] * inv[:, None]
    ang_c = col[None, :] * inv[:, None]
    cosT = np.concatenate([np.cos(ang_r), np.cos(ang_r), np.cos(ang_c), np.cos(ang_c)], 0)
    sinT = np.concatenate([np.sin(ang_r), np.sin(ang_r), np.sin(ang_c), np.sin(ang_c)], 0)
    ohv = np.zeros((33, 3, 384), np.float32)
    for gi, (_, dil) in enumerate(A_GROUPS):
        for xi in range(384):
            rel = xi - 127 - 64
            if xi < 383 and abs(rel) <= 64:
                ohv[int(_t5_bucket(np.array((-rel if rev else rel) * dil))), gi, xi] = 1.0
            else:
                ohv[32, gi, xi] = NEG
    c["ohv"] = ohv
    cs = np.zeros((NE, NE * 128), np.float32)
    for e in range(NE):
        cs[e, e * 128:(e + 1) * 128] = 1.0
    c["csel"] = cs
    c["antiI"] = np.ascontiguousarray(np.eye(128, dtype=np.float32)[::-1])
    eb = np.zeros((128, 3), np.float32)
    eb[:64, 1] = NEG
    eb[64:, 2] = NEG
    c["edge"] = eb
    c["cosT"] = cosT.astype(np.float32)
    c["sinT"] = sinT.astype(np.float32)
    return c


class Phase:
    def __init__(self, nc, name):
        self.nc = nc
        self.name = name
        self.sc = Sched()
        self.st = contextlib.ExitStack()
        self.n = 0
        self.finals = []

    CONST_TAGS = ("rb", "oh", "ones", "ropeP", "gqk", "bg", "eps", "tab", "antiI", "edge", "wa", "wb", "wo", "lng",
                  "lnb", "ident", "wr", "br", "bl", "bd", "csel", "identf")

    def sb(self, shape, dt, tag="t"):
        self.n += 1
        t = self.st.enter_context(self.nc.sbuf_tensor(f"{self.name}_{tag}{self.n}", list(shape), dt))
        if tag in self.CONST_TAGS:
            if not hasattr(self, "cres"):
                self.cres = Res("const")
            return t, self.cres
        return t, Res(f"{tag}{self.n}")

    def ps(self, shape=(128, 512), dt=F32, tag="ps"):
        self.n += 1
        t = self.st.enter_context(self.nc.psum_tensor(f"{self.name}_{tag}{self.n}", list(shape), dt))
        return t, Res(f"{tag}{self.n}")

    def mm(self, out, lhsT, rhs, start, stop, reads, writes):
        self.sc.op("pe", lambda h: h.matmul(out, lhsT, rhs, start=start, stop=stop), reads, writes)

    def tr(self, out, in_, ident, reads, writes):
        self.sc.op("pe", lambda h: h.transpose(out, in_, ident), reads, writes)

    def act(self, out, in_, func, reads, writes, bias=None, scale=None, accum_out=None):
        kw = {}
        if bias is not None:
            kw["bias"] = bias
        if scale is not None:
            kw["scale"] = scale
        if accum_out is not None:
            kw["accum_out"] = accum_out
        self.sc.op("act", lambda h: h.activation(out, in_, func, **kw), reads, writes)

    def cp(self, eng, out, in_, reads, writes):
        if eng == "act":
            self.sc.op("act", lambda h: h.copy(out, in_), reads, writes)
        else:
            self.sc.op(eng, lambda h: h.tensor_copy(out, in_), reads, writes)

    def ts(self, eng, out, in0, s1, s2, op0, op1, reads, writes, accum_out=None):
        if op1 is None:
            self.sc.op(eng, lambda h: h.tensor_scalar(out, in0, s1, None, op0), reads, writes)
        elif accum_out is not None:
            self.sc.op(eng, lambda h: h.tensor_scalar(out, in0, s1, s2, op0, op1, accum_out), reads, writes)
        else:
            self.sc.op(eng, lambda h: h.tensor_scalar(out, in0, s1, s2, op0, op1), reads, writes)

    def tt(self, eng, out, in0, in1, op, reads, writes):
        self.sc.op(eng, lambda h: h.tensor_tensor(out, in0, in1, op), reads, writes)

    def stt(self, eng, out, in0, scalar, in1, op0, op1, reads, writes):
        self.sc.op(eng, lambda h: h.scalar_tensor_tensor(out, in0, scalar, in1, op0, op1), reads, writes)

    def memset(self, eng, ap, val, writes):
        self.sc.op(eng, lambda h: h.memset(ap, val), (), writes)

    def load(self, queue, out, in_, dsem, writes, reads=()):
        if writes and writes[0] is getattr(self, "cres", None):
            dsem = "c"
        return self.sc.dma(queue, lambda h: h.dma_start(out=out, in_=in_), dsem, reads, writes)

    def store(self, queue, out, in_, dsem, reads, writes=()):
        ev = self.sc.dma(queue, lambda h: h.dma_start(out=out, in_=in_), dsem, reads, writes)
        self.finals.append(ev)
        return ev

    def close(self):
        last = {}
        for e in ENGS:
            for ins in self.sc.streams[e]:
                last[ins.ev.eng] = ins.ev
        for ev in last.values():
            ev.marked = True
        self.sc.emit(self.nc, list(last.values()), self.name)
        self.st.close()


def _dram(nc, name, shape, dt, kind):
    return nc.dram_tensor(name, list(shape), dt, kind=kind).ap()


def phase1(nc, I, T, n_tb=NTB):
    ph = Phase(nc, "p1")
    wbl = [ph.sb([128, 16, 512], BF16, "w") for _ in range(2)]
    xbl = [ph.sb([128, 16, 512], BF16, "x") for _ in range(2)]
    obl = [ph.sb([128, 4, 512], BF16, "o") for _ in range(2)]
    banks = [ph.ps() for _ in range(8)]
    ones_t, ones_r = ph.sb([128, 128], BF16, "ones")
    rp_t, rp_r = ph.sb([128, 128], BF16, "ropeP")
    gq_t, gq_r = ph.sb([128, 2], F32, "gqk")
    bg_t, bg_r = ph.sb([128, 32], F32, "bg")
    cs = [ph.sb([128, 2, 512], F32, "cs") for _ in range(2)]
    sq = [ph.sb([128, 512], BF16, "sq") for _ in range(2)]
    r0 = [ph.sb([128, 512], F32, "r0") for _ in range(2)]
    kn = [ph.sb([128, 512], BF16, "kn") for _ in range(2)]
    ta = [ph.sb([128, 512], F32, "ta") for _ in range(2)]
    tb_ = [ph.sb([128, 512], F32, "tb") for _ in range(2)]

    eps_t, eps_r = ph.sb([128, 1], F32, "eps")
    ph.memset("dve", eps_t[:], 128.0 * QK_EPS, [eps_r])
    ph.load("pool", ones_t[:], I["c_ones"][:, :], "c1", [ones_r])
    ph.load("pool", rp_t[:], I["c_ropeP"][:, :], "c2", [rp_r])
    ph.load("sp", gq_t[:], I["qk_normT"][:, :], "c3", [gq_r])
    ph.load("sp", bg_t[:], I["b_gatesT"][:, :], "c4", [bg_r])
    ph.ts("dve", gq_t[:], gq_t[:], math.sqrt(128.0), None, ALU.mult, None, [gq_r], [gq_r])

    bank_i = [0]
    xcnt = [0]
    ocnt = [0]
    rcnt = [0]
    cpe = [0]

    def next_bank():
        b = banks[bank_i[0] % 8]
        bank_i[0] += 1
        return b

    def fm_chunk(wt, wr, xt, xr, j):
        pt, pr = next_bank()
        for dc in range(16):
            ph.mm(pt[:], wt[:, dc, j * 128:(j + 1) * 128], xt[:, dc, :], dc == 0, dc == 15, [wr, xr], [pr])
        return pt, pr

    def rope_epilogue(pt, pr, gcol, cst, csr, out_ap, out_r):
        i = rcnt[0] % 2
        rcnt[0] += 1
        sq_t, sq_r = sq[i]
        r0_t, r0_r = r0[i]
        kn_t, kn_r = kn[i]
        ta_t, ta_r = ta[i]
        tb_t, tb_r = tb_[i]
        ph.act(sq_t[:], pt[:], AF.Square, [pr], [sq_r])
        p2, p2r = next_bank()
        ph.mm(p2[:], ones_t[:], sq_t[:], True, True, [ones_r, sq_r], [p2r])
        ph.act(tb_t[:], p2[:], AF.Sqrt, [p2r, eps_r], [tb_r], bias=eps_t[:, 0:1], scale=1.0)
        ph.sc.op("dve", (lambda h_, a=r0_t, b=tb_t: h_.reciprocal(a[:], b[:])), [tb_r], [r0_r])
        ph.stt("dve", kn_t[:], pt[:], gq_t[:, gcol:gcol + 1], r0_t[:], ALU.mult, ALU.mult, [pr, gq_r, r0_r], [kn_r])
        p3, p3r = next_bank()
        ph.mm(p3[:], rp_t[:], kn_t[:], True, True, [rp_r, kn_r], [p3r])
        ph.tt("dve", ta_t[:], kn_t[:], cst[:, 0, :], ALU.mult, [kn_r, csr], [ta_r])
        ph.tt("dve", tb_t[:], p3[:], cst[:, 1, :], ALU.mult, [p3r, csr], [tb_r])
        ph.tt("dve", out_ap, ta_t[:], tb_t[:], ALU.add, [ta_r, tb_r], [out_r])

    for g in range(20):
        wt, wr = wbl[g % 2]
        ph.load("pool", wt[:], I["w_in"][0, :, g * 512:(g + 1) * 512].rearrange("(c p) f -> p c f", p=128), f"w{g % 2}", [wr])
        own_only = g < 3 or g in (9, 10) or g >= 12
        n_blk = n_tb if own_only else (min(NTB, n_tb + 2) if 3 <= g <= 8 else NTB)
        for tb in range(n_blk):
            xs = xcnt[0] % 2
            xcnt[0] += 1
            xt, xr = xbl[xs]
            ph.load("pool", xt[:], I["xT"][:, tb * TB:(tb + 1) * TB].rearrange("(c p) t -> p c t", p=128), f"x{xs}", [xr])
            os_ = ocnt[0] % 2
            ocnt[0] += 1
            ot, orr = obl[os_]
            tsl = slice(tb * TB, (tb + 1) * TB)
            need_cs = g in (9, 10, 11)
            if need_cs:
                cst, csr = cs[tb % 2]
                ph.load("sp", cst[:, 0, :], I["c_cosT"][:, tsl], f"cs{tb % 2}", [csr])
                ph.load("sp", cst[:, 1, :], I["c_sinT"][:, tsl], f"cs{tb % 2}", [csr])
            if g < 6 or g >= 12:
                for j in range(4):
                    pt, pr = fm_chunk(wt, wr, xt, xr, j)
                    if g >= 12:
                        fc = (g - 12) * 4 + j
                        ph.act(ot[:, j, :], pt[:], AF.Sigmoid, [pr, bg_r], [orr], bias=bg_t[:, fc:fc + 1])
                    else:
                        eng = "act" if cpe[0] % 2 == 0 else "dve"
                        cpe[0] += 1
                        ph.cp(eng, ot[:, j, :], pt[:], [pr], [orr])
                if g < 3:
                    dst = T["QAT"][g * 4:(g + 1) * 4, :, tsl]
                elif g < 6:
                    dst = T["KAT"][(g - 3) * 4:(g - 2) * 4, :, tsl]
                else:
                    dst = T["GT"][(g - 12) * 4:(g - 11) * 4, :, tsl]
                ph.store("sp", dst.rearrange("c p t -> p c t"), ot[:], f"o{os_}", [orr])
            elif g in (9, 10):
                for j in range(4):
                    pt, pr = fm_chunk(wt, wr, xt, xr, j)
                    rope_epilogue(pt, pr, 0, cst, csr, ot[:, j, :], orr)
                dst = T["QBT"][(g - 9) * 4:(g - 8) * 4, :, tsl]
                ph.store("sp", dst.rearrange("c p t -> p c t"), ot[:], f"o{os_}", [orr])
            elif g in (6, 7, 8):
                for tc in range(4):
                    pt, pr = next_bank()
                    for dc in range(16):
                        ph.mm(pt[:], xt[:, dc, tc * 128:(tc + 1) * 128], wt[:, dc, :], dc == 0, dc == 15, [wr, xr], [pr])
                    eng = "act" if cpe[0] % 2 == 0 else "dve"
                    cpe[0] += 1
                    ph.cp(eng, ot[:, tc, :], pt[:], [pr], [orr])
                dst = T["VA"][tsl, (g - 6) * 512:(g - 5) * 512]
                ph.store("sp", dst.rearrange("(c p) f -> p c f", p=128), ot[:], f"o{os_}", [orr])
            else:
                for j in range(2):
                    pt, pr = fm_chunk(wt, wr, xt, xr, j)
                    rope_epilogue(pt, pr, 1, cst, csr, ot[:, j, :], orr)
                dst = T["KBT"][0:2, :, tsl]
                ph.store("sp", dst.rearrange("c p t -> p c t"), ot[:, 0:2, :], f"o{os_}", [orr])
                os2 = ocnt[0] % 2
                ocnt[0] += 1
                ot2, orr2 = obl[os2]
                for tc in range(4):
                    pt, pr = next_bank()
                    for dc in range(16):
                        ph.mm(pt[:, 0:256], xt[:, dc, tc * 128:(tc + 1) * 128], wt[:, dc, 256:512], dc == 0, dc == 15, [wr, xr], [pr])
                    ph.cp("act", ot2[:, tc, 0:256], pt[:, 0:256], [pr], [orr2])
                dst = T["VB"][tsl, :]
                ph.store("sp", dst.rearrange("(c p) f -> p c f", p=128), ot2[:, :, 0:256], f"o{os2}", [orr2])
    ph.close()


def phase2b(nc, I, T, n_tb=NTB):
    ph = Phase(nc, "p2b")
    kt_t, kt_r = ph.sb([128, S], BF16, "kT")
    v_t, v_r = ph.sb([128, 64, 128], BF16, "v")
    ones_t, ones_r = ph.sb([128, 128], BF16, "ones")
    qb = [ph.sb([128, TB], BF16, "q") for _ in range(2)]
    pb = [ph.sb([128, TB], BF16, "p") for _ in range(3)]
    ob = [ph.sb([128, TB], BF16, "o") for _ in range(2)]
    rc = [ph.sb([128, TB], F32, "rc") for _ in range(2)]
    sbank = [ph.ps() for _ in range(3)]
    obank = [ph.ps() for _ in range(2)]
    dbank = [ph.ps() for _ in range(2)]
    ph.load("pool", ones_t[:], I["c_ones"][:, :], "c1", [ones_r])
    scale = 1.0 / math.sqrt(HD)
    u = 0
    pc = 0
    for kvh in range(2):
        ph.load("sp", kt_t[:], T["KBT"][kvh, :, :], "kt", [kt_r])
        ph.load("sp", v_t[:], T["VB"][:, kvh * 128:(kvh + 1) * 128].rearrange("(c p) f -> p c f", p=128), "v", [v_r])
        for hh in range(4):
            h = kvh * 4 + hh
            for qi in range(n_tb):
                qt, qr = qb[u % 2]
                ot, orr = ob[u % 2]
                rt, rr = rc[u % 2]
                op_, opr = obank[u % 2]
                dp, dpr = dbank[u % 2]
                tsl = slice(qi * TB, (qi + 1) * TB)
                ph.load("sp", qt[:], T["QBT"][h, :, tsl], f"q{u % 2}", [qr])
                for kc in range(64):
                    sp_, spr = sbank[pc % 3]
                    pt, pr = pb[pc % 3]
                    pc += 1
                    ph.mm(sp_[:], kt_t[:, kc * 128:(kc + 1) * 128], qt[:], True, True, [kt_r, qr], [spr])
                    ph.act(pt[:], sp_[:], AF.Exp, [spr], [pr], scale=scale)
                    ph.mm(op_[:], v_t[:, kc, :], pt[:], kc == 0, kc == 63, [v_r, pr], [opr])
                    ph.mm(dp[:], ones_t[:], pt[:], kc == 0, kc == 63, [ones_r, pr], [dpr])
                ph.sc.op("dve", (lambda h_, a=rt, b=dp: h_.reciprocal(a[:], b[:])), [dpr], [rr])
                ph.tt("dve", ot[:], op_[:], rt[:], ALU.mult, [opr, rr], [orr])
                ph.store("sp", T["YBT"][h, :, tsl], ot[:], f"o{u % 2}", [orr])
                u += 1
    ph.close()


SCRATCH = {
    "QAT": ([12, 128, S], BF16), "KAT": ([12, 128, S], BF16), "VA": ([S, 1536], BF16),
    "QBT": ([8, 128, S], BF16), "KBT": ([2, 128, S], BF16), "VB": ([S, 256], BF16),
    "GT": ([32, 128, S], BF16), "YBT": ([8, 128, S], BF16), "YAT": ([4, 128, S], BF16),
    "X1S": ([S, D], F32), "X1TS": ([16, 128, S], BF16), "U": ([12, 512], F32), "YT": ([16, 128, S], BF16), "FT": ([16, 128, S], F32), "X1": ([S, D], F32), "X1T": ([16, 128, S], BF16),
}


def build(phases=("1", "2b"), dbg=(), n_tb=NTB, n_e=NE, n_sel=None):
    if n_sel is None:
        n_sel = n_tb
    nc = bass.Bass("TRN2", target_bir_lowering=False)
    nc._k_sem_stack = contextlib.ExitStack()
    nc._k_csem = {}
    nc._k_dsem = {}
    I = {}
    hc = _host_consts()

    def din(name, shape):
        I[name] = _dram(nc, name, shape, F32, "ExternalInput")

    din("xT", [D, S])
    din("x", [S, D])
    din("w_in", [1, D, IN_W])
    din("qk_normT", [128, 2])
    din("b_gatesT", [128, 32])
    din("rel_bias", [32, 12])
    din("w_branch_a", [1, 512, D])
    din("w_branch_b", [1, 1024, D])
    din("w_out", [1, D, D])
    for nm in ("ln1_g", "ln1_b", "ln2_g", "ln2_b"):
        din(nm, [1, D])
    din("w_router", [1, D, NE])
    din("b_router", [1, NE])
    din("b_gateT", [128, NE, 16])
    din("b_linT", [128, NE, 16])
    din("b_down", [1, NE, D])
    for nm in ("w_gate", "w_lin", "w_down"):
        din(nm, [1, n_e, D, D])
    out_ap = _dram(nc, "out", [n_sel * TB, D], F32, "ExternalOutput")
    din("sel", [n_tb * TB, n_sel * TB])
    for k, v in hc.items():
        din("c_" + k, list(v.shape))
    T = {}
    for k, (shape, dt) in SCRATCH.items():
        T[k] = _dram(nc, "s_" + k, shape, dt, "ExternalOutput" if k in dbg else "Internal")
    if "0" in phases:
        phase0(nc, I, T)
    if "1" in phases:
        phase1(nc, I, T, n_tb)
    if "2a" in phases:
        phase2a(nc, I, T, n_tb)
    if "2b" in phases:
        phase2b(nc, I, T, n_tb)
    if "3a" in phases:
        phase3a(nc, I, T, n_tb)
    if "3b" in phases:
        phase3b(nc, I, T, n_tb)
    if "WT" in dbg:
        T["WT"] = _dram(nc, "s_WT", [NE, S], F32, "ExternalOutput")
    if "S" in phases:
        phaseS(nc, I, T, n_tb, n_sel)
    if "4" in phases:
        phase4(nc, I, T, n_sel, n_e)
    if "5" in phases:
        phase5(nc, I, T, out_ap, n_sel)
    nc._k_sem_stack.close()
    return nc


def host_inputs(inp, n_e=NE, rev=False, n_half=NTB, n_sel=None, q=0):
    m = {}
    x = np.asarray(inp["x"], np.float32)[0]
    if rev:
        x = x[::-1]
    m["x"] = np.ascontiguousarray(x)
    m["xT"] = np.ascontiguousarray(x.T)
    m["w_in"] = np.asarray(inp["w_in"], np.float32)
    m["qk_normT"] = np.ascontiguousarray(np.stack([np.asarray(inp["q_norm"])[0], np.asarray(inp["k_norm"])[0]], 1).astype(np.float32))
    m["b_gatesT"] = np.ascontiguousarray(np.asarray(inp["b_gates"], np.float32)[0].reshape(32, 128).T)
    for nm in ("w_branch_a", "w_branch_b", "w_out", "ln1_g", "ln1_b", "ln2_g", "ln2_b"):
        m[nm] = np.ascontiguousarray(np.asarray(inp[nm], np.float32))
    for nm in ("w_router", "b_router", "b_down"):
        m[nm] = np.asarray(inp[nm], np.float32)
    for nm in ("w_gate", "w_lin", "w_down"):
        m[nm] = np.asarray(inp[nm], np.float32)[:, :n_e]
    m["b_gateT"] = np.ascontiguousarray(np.asarray(inp["b_gate"], np.float32)[0].reshape(NE, 16, 128).transpose(2, 0, 1))
    m["b_linT"] = np.ascontiguousarray(np.asarray(inp["b_lin"], np.float32)[0].reshape(NE, 16, 128).transpose(2, 0, 1))
    m["rel_bias"] = np.ascontiguousarray(np.asarray(inp["rel_bias"], np.float32))
    for k, v in _host_consts(rev).items():
        m["c_" + k] = v
    if n_sel is None:
        n_sel = n_half
    sel = np.zeros((n_half * TB, n_sel * TB), np.float32)
    j = np.arange(n_sel * TB)
    sel[q * n_sel * TB + j, j] = 1.0
    m["sel"] = sel
    return m


def phase0(nc, I, T):
    ph = Phase(nc, "p0")
    rb_t, rb_r = ph.sb([33, 12], F32, "rb")
    oh_t, oh_r = ph.sb([33, 3, 384], F32, "oh")
    u_t, u_r = ph.sb([4, 3, 384], F32, "u")
    ph.memset("dve", rb_t[32:33, :], 1.0, [rb_r])
    ph.load("sp", rb_t[0:32, :], I["rel_bias"][:, :], "c1", [rb_r])
    ph.load("sp", oh_t[:], I["c_ohv"][:, :, :], "c2", [oh_r])
    for g in range(3):
        pt, pr = ph.ps([4, 384])
        ph.mm(pt[:], rb_t[:, 4 * g:4 * g + 4], oh_t[:, g, :], True, True, [rb_r, oh_r], [pr])
        ph.cp("dve", u_t[:, g, :], pt[:], [pr], [u_r])
        ph.store("sp", T["U"][4 * g:4 * g + 4, 0:384], u_t[:, g, :], f"o{g}", [u_r])
    ph.close()


def phase2a(nc, I, T, n_tb=NTB):
    ph = Phase(nc, "p2a")
    oacc_t, oacc_r = ph.sb([128, S], F32, "oacc")
    dacc_t, dacc_r = ph.sb([128, S], F32, "dacc")
    tab_t, tab_r = ph.sb([128, 12, 2, 128], F32, "tab")
    hk = [ph.sb([128, 128], F32, "hk") for _ in range(2)]
    aI_t, aI_r = ph.sb([128, 128], F32, "antiI")
    ones_t, ones_r = ph.sb([128, 128], BF16, "ones")
    eb_t, eb_r = ph.sb([128, 3], F32, "edge")
    kw = [ph.sb([128, 4096], BF16, "kw") for _ in range(2)]
    qw = [ph.sb([128, 2048], BF16, "qw") for _ in range(2)]
    vw = [ph.sb([128, 16, 2, 128], BF16, "vw") for _ in range(2)]
    tmp = [ph.sb([128, 128], F32, "tmp") for _ in range(3)]
    pb = [ph.sb([128, 128], BF16, "p") for _ in range(3)]
    rc_t, rc_r = ph.sb([128, 2048], F32, "rc")
    yb = [ph.sb([128, 2048], BF16, "y") for _ in range(2)]
    sbank = [ph.ps([128, 128]) for _ in range(3)]
    obank = [ph.ps([128, 128]) for _ in range(2)]
    dbank = [ph.ps([128, 128]) for _ in range(2)]
    tbank = ph.ps([128, 128])

    ph.load("sp", aI_t[:], I["c_antiI"][:, :], "c1", [aI_r])
    ph.load("pool", ones_t[:], I["c_ones"][:, :], "c2", [ones_r])
    ph.load("sp", eb_t[:], I["c_edge"][:, :], "c3", [eb_r])
    for i in range(2):
        ph.memset("pool", kw[i][0][:], 0.0, [kw[i][1]])
        ph.memset("pool", vw[i][0][:], 0.0, [vw[i][1]])
    n = 0
    for hd_ in range(12):
        for c in range(2):
            ht, hr = hk[n % 2]
            n += 1
            src = bass.AP(T["U"].tensor, hd_ * 512 + c * 128, [[1, 128], [1, 128]])
            ph.load("sp", ht[:], src, f"hk{n % 2}", [hr])
            ph.mm(tbank[0][:], ht[:], aI_t[:], True, True, [hr, aI_r], [tbank[1]])
            ph.cp("dve", tab_t[:, hd_, c, :], tbank[0][:], [tbank[1]], [tab_r])

    scale = 1.0 / math.sqrt(HD)
    wn = 0
    pc = 0
    un = 0
    for j in range(4):
        for g, (_, d) in enumerate(A_GROUPS):
            head = 4 * g + j
            span = 128 * d
            nb = S // span
            var = T["VA"].rearrange("(n d) f -> d n f", d=d)
            for b in range(nb * n_tb // NTB):
                base = b * span
                kt, kr = kw[wn % 2]
                qt, qr = qw[wn % 2]
                vt, vr = vw[wn % 2]
                wn += 1
                lo = base - 64 * d
                hi = base + 192 * d
                clo, chi = max(lo, 0), min(hi, S)
                ph.load("sp", kt[:, clo - lo:chi - lo], T["KAT"][head, :, clo:chi], f"kw{wn % 2}", [kr])
                ph.load("sp", qt[:, 0:span], T["QAT"][head, :, base:base + span], f"qw{wn % 2}", [qr])
                n0 = base // d - 64
                for c in range(2):
                    p0, p1 = 0, 128
                    if n0 + c * 128 < 0:
                        p0 = -(n0 + c * 128)
                    if n0 + (c + 1) * 128 > S // d:
                        p1 = S // d - (n0 + c * 128)
                    src = var[:, n0 + c * 128 + p0:n0 + c * 128 + p1, head * 128:(head + 1) * 128].rearrange("r p f -> p r f")
                    ph.load("pool", vt[p0:p1, 0:d, c, :], src, f"vw{wn % 2}", [vr])
                for r in range(d):
                    op_, opr = obank[un % 2]
                    dp, dpr = dbank[un % 2]
                    un += 1
                    for c in range(2):
                        sp_, spr = sbank[pc % 3]
                        tt_, ttr = tmp[pc % 3]
                        pt, pr = pb[pc % 3]
                        pc += 1
                        k0 = r + c * 128 * d
                        ph.mm(sp_[:], kt[:, k0:k0 + 127 * d + 1:d], qt[:, r:r + 127 * d + 1:d], True, True, [kr, qr], [spr])
                        ph.stt("dve", tt_[:], sp_[:], scale, tab_t[:, head, c, :], ALU.mult, ALU.add, [spr, tab_r], [ttr])
                        ecol = 0
                        if b == 0 and c == 0:
                            ecol = 1
                        if b == nb - 1 and c == 1:
                            ecol = 2
                        ph.act(pt[:], tt_[:], AF.Exp, [ttr, eb_r], [pr], bias=eb_t[:, ecol:ecol + 1])
                        ph.mm(op_[:], vt[:, r, c, :], pt[:], c == 0, c == 1, [vr, pr], [opr])
                        ph.mm(dp[:], ones_t[:], pt[:], c == 0, c == 1, [ones_r, pr], [dpr])
                    osl = oacc_t[:, base + r:base + r + 127 * d + 1:d]
                    dsl = dacc_t[:, base + r:base + r + 127 * d + 1:d]
                    if g == 0:
                        ph.cp("dve", osl, op_[:], [opr], [oacc_r])
                        ph.cp("pool" if False else "dve", dsl, dp[:], [dpr], [dacc_r])
                    else:
                        ph.tt("dve", osl, osl, op_[:], ALU.add, [opr, oacc_r], [oacc_r])
                        ph.tt("dve", dsl, dsl, dp[:], ALU.add, [dpr, dacc_r], [dacc_r])
        for seg in range(4 * n_tb // NTB):
            yt, yr = yb[seg % 2]
            ssl = slice(seg * 2048, (seg + 1) * 2048)
            ph.sc.op("dve", (lambda h_, a=rc_t, b_=dacc_t, sl=ssl: h_.reciprocal(a[:], b_[:, sl])), [dacc_r], [rc_r])
            ph.tt("dve", yt[:], oacc_t[:, ssl], rc_t[:], ALU.mult, [oacc_r, rc_r], [yr])
            ph.store("sp", T["YAT"][j, :, ssl], yt[:], f"y{seg % 2}", [yr])
    ph.close()


def phase3a(nc, I, T, n_tb=NTB):
    ph = Phase(nc, "p3a")
    wa_t, wa_r = ph.sb([128, 4, D], BF16, "wa")
    wb_t, wb_r = ph.sb([128, 8, D], BF16, "wb")
    ya = [ph.sb([128, 4, TB], BF16, "ya") for _ in range(2)]
    yb = [ph.sb([128, 8, TB], BF16, "yb") for _ in range(2)]
    gt = [ph.sb([128, 2, TB], BF16, "g") for _ in range(3)]
    t1 = [ph.sb([128, TB], F32, "t1") for _ in range(2)]
    t2 = [ph.sb([128, TB], F32, "t2") for _ in range(2)]
    yo = [ph.sb([128, 4, TB], BF16, "yo") for _ in range(2)]
    pa = [ph.ps() for _ in range(2)]
    pb = [ph.ps() for _ in range(2)]
    ph.load("pool", wa_t[:], I["w_branch_a"][0].rearrange("(c p) f -> p c f", p=128), "c1", [wa_r])
    ph.load("pool", wb_t[:], I["w_branch_b"][0].rearrange("(c p) f -> p c f", p=128), "c2", [wb_r])
    n = 0
    on = 0
    for tb in range(n_tb):
        tsl = slice(tb * TB, (tb + 1) * TB)
        yat, yar = ya[tb % 2]
        ybt, ybr = yb[tb % 2]
        ph.load("sp", yat[:], T["YAT"][:, :, tsl].rearrange("c p t -> p c t"), f"ya{tb % 2}", [yar])
        ph.load("sp", ybt[:], T["YBT"][:, :, tsl].rearrange("c p t -> p c t"), f"yb{tb % 2}", [ybr])
        for fc in range(16):
            g_t, g_r = gt[n % 3]
            ph.load("sp", g_t[:, 0, :], T["GT"][fc, :, tsl], f"g{n % 3}", [g_r])
            ph.load("sp", g_t[:, 1, :], T["GT"][16 + fc, :, tsl], f"g{n % 3}", [g_r])
            pat, par = pa[n % 2]
            pbt, pbr = pb[n % 2]
            t1t, t1r = t1[n % 2]
            t2t, t2r = t2[n % 2]
            n += 1
            for j in range(4):
                ph.mm(pat[:], wa_t[:, j, fc * 128:(fc + 1) * 128], yat[:, j, :], j == 0, j == 3, [wa_r, yar], [par])
            for j in range(8):
                ph.mm(pbt[:], wb_t[:, j, fc * 128:(fc + 1) * 128], ybt[:, j, :], j == 0, j == 7, [wb_r, ybr], [pbr])
            if fc % 4 == 0:
                yot, yor = yo[on % 2]
                on += 1
            ph.tt("dve", t1t[:], pat[:], g_t[:, 0, :], ALU.mult, [par, g_r], [t1r])
            ph.tt("dve", t2t[:], pbt[:], g_t[:, 1, :], ALU.mult, [pbr, g_r], [t2r])
            ph.tt("pool", yot[:, fc % 4, :], t1t[:], t2t[:], ALU.add, [t1r, t2r], [yor])
            if fc % 4 == 3:
                ph.store("sp", T["YT"][fc - 3:fc + 1, :, tsl].rearrange("c p t -> p c t"), yot[:], f"yo{(on - 1) % 2}", [yor])
    ph.close()


def _layer_norm(ph, h_t, h_r, out_t, out_r, g_t, g_r, b_t, b_r, st, eps_t, eps_r, junk_t, junk_r):
    s_t, s_r = st
    ph.memset("dve", s_t[:], 0.0, [s_r])
    ph.act(junk_t[:], h_t[:], AF.Copy, [h_r, s_r], [junk_r, s_r], accum_out=s_t[:, 0:1])
    ph.act(junk_t[:], h_t[:], AF.Square, [h_r, s_r], [junk_r, s_r], accum_out=s_t[:, 1:2])
    ph.ts("dve", s_t[:, 2:3], s_t[:, 0:1], 1.0 / D, None, ALU.mult, None, [s_r], [s_r])
    ph.tt("dve", s_t[:, 3:4], s_t[:, 2:3], s_t[:, 2:3], ALU.mult, [s_r], [s_r])
    ph.stt("dve", s_t[:, 4:5], s_t[:, 1:2], 1.0 / D, s_t[:, 3:4], ALU.mult, ALU.subtract, [s_r], [s_r])
    ph.act(s_t[:, 5:6], s_t[:, 4:5], AF.Sqrt, [s_r, eps_r], [s_r], bias=eps_t[:, 0:1], scale=1.0)
    ph.sc.op("dve", (lambda h_, a=s_t: h_.reciprocal(a[:, 6:7], a[:, 5:6])), [s_r], [s_r])
    ph.ts("dve", out_t[:], h_t[:], s_t[:, 2:3], s_t[:, 6:7], ALU.subtract, ALU.mult, [h_r, s_r], [out_r])
    ph.tt("pool", out_t[:], out_t[:], g_t[:], ALU.mult, [out_r, g_r], [out_r])
    ph.tt("pool", out_t[:], out_t[:], b_t[:], ALU.add, [out_r, b_r], [out_r])


def phase3b(nc, I, T, n_tb=NTB):
    ph = Phase(nc, "p3b")
    wo_t, wo_r = ph.sb([128, 16, D], BF16, "wo")
    g_t, g_r = ph.sb([128, D], F32, "lng")
    b_t, b_r = ph.sb([128, D], F32, "lnb")
    id_t, id_r = ph.sb([128, 128], BF16, "ident")
    eps_t, eps_r = ph.sb([128, 1], F32, "eps")
    yt = [ph.sb([128, 16, TB], BF16, "yT") for _ in range(2)]
    xc = [ph.sb([128, D], F32, "x") for _ in range(2)]
    h1 = [ph.sb([128, D], F32, "h1") for _ in range(2)]
    x1 = [ph.sb([128, D], F32, "x1") for _ in range(2)]
    x1b = [ph.sb([128, D], BF16, "x1b") for _ in range(2)]
    junk_t, junk_r = ph.sb([128, D], BF16, "junk")
    xT = [ph.sb([128, 16, TB], BF16, "x1T") for _ in range(2)]
    stt_ = [ph.sb([128, 8], F32, "st") for _ in range(2)]
    mixb = [ph.ps() for _ in range(4)]
    trb = [ph.ps([128, 512], BF16, "tr") for _ in range(2)]
    ph.memset("dve", eps_t[:], LN_EPS, [eps_r])
    ph.load("pool", wo_t[:], I["w_out"][0].rearrange("(c p) f -> p c f", p=128), "c1", [wo_r])
    ph.load("sp", g_t[:], I["ln1_g"].partition_broadcast(128), "c2", [g_r])
    ph.load("sp", b_t[:], I["ln1_b"].partition_broadcast(128), "c3", [b_r])
    ph.load("pool", id_t[:], I["c_ident"][:, :], "c4", [id_r])
    n = 0
    trn = 0
    for tb in range(n_tb):
        tsl = slice(tb * TB, (tb + 1) * TB)
        ytt, ytr = yt[tb % 2]
        ph.load("sp", ytt[:], T["YT"][:, :, tsl].rearrange("c p t -> p c t"), f"yt{tb % 2}", [ytr])
        xTt, xTr = xT[tb % 2]
        for tc in range(4):
            r0 = tb * TB + tc * 128
            xct, xcr = xc[n % 2]
            h1t, h1r = h1[n % 2]
            x1t, x1r = x1[n % 2]
            x1bt, x1br = x1b[n % 2]
            st = stt_[n % 2]
            ph.load("sp", xct[:], I["x"][r0:r0 + 128, :], f"x{n % 2}", [xcr])
            for nb in range(4):
                mt, mr = mixb[nb]
                for fc in range(16):
                    ph.mm(mt[:], ytt[:, fc, tc * 128:(tc + 1) * 128], wo_t[:, fc, nb * 512:(nb + 1) * 512], fc == 0, fc == 15, [ytr, wo_r], [mr])
                ph.stt("dve", h1t[:, nb * 512:(nb + 1) * 512], xct[:, nb * 512:(nb + 1) * 512], ALPHA, mt[:], ALU.mult, ALU.add, [xcr, mr], [h1r])
            _layer_norm(ph, h1t, h1r, x1t, x1r, g_t, g_r, b_t, b_r, st, eps_t, eps_r, junk_t, junk_r)
            ph.store("sp", T["X1"][r0:r0 + 128, :], x1t[:], f"x1o{n % 2}", [x1r])
            ph.cp("act", x1bt[:], x1t[:], [x1r], [x1br])
            for q in range(4):
                tt_, ttr = trb[trn % 2]
                trn += 1
                for k in range(4):
                    dc = q * 4 + k
                    ph.tr(tt_[:, k * 128:(k + 1) * 128], x1bt[:, dc * 128:(dc + 1) * 128], id_t[:], [x1br, id_r], [ttr])
                ph.cp("dve" if q % 2 else "act", xTt[:, q * 4:(q + 1) * 4, tc * 128:(tc + 1) * 128],
                      tt_[:].rearrange("p (k t) -> p k t", k=4), [ttr], [xTr])
            n += 1
        ph.store("sp", T["X1T"][:, :, tsl].rearrange("c p t -> p c t"), xTt[:], f"xT{tb % 2}", [xTr])
    ph.close()


def phase4(nc, I, T, n_tb=NTB, n_e=NE):
    ph = Phase(nc, "p4")
    wr_t, wr_r = ph.sb([128, 16, NE], BF16, "wr")
    br_t, br_r = ph.sb([128, NE], F32, "br")
    bg_t, bg_r = ph.sb([128, NE, 16], F32, "bg")
    bl_t, bl_r = ph.sb([128, NE, 16], F32, "bl")
    bd_t, bd_r = ph.sb([NE, D], F32, "bd")
    c_t, c_r = ph.sb([NE, NE * 128], F32, "csel")
    idf_t, idf_r = ph.sb([128, 128], F32, "identf")
    xT_t, xT_r = ph.sb([128, 16, TB], BF16, "x1T")
    hT_t, hT_r = ph.sb([128, 16, TB], BF16, "hT")
    acc_t, acc_r = ph.sb([128, 16, TB], F32, "acc")
    wT_t, wT_r = ph.sb([NE, TB], F32, "WT")
    ring = [ph.sb([128, 16, 512], BF16, "ring") for _ in range(4)]
    lg = [ph.sb([128, NE], F32, "lg") for _ in range(2)]
    t8 = [ph.sb([128, 8], F32, "t8") for _ in range(2)]
    sm = [ph.sb([128, 4], F32, "sm") for _ in range(2)]
    mk = [ph.sb([128, NE], F32, "mk") for _ in range(2)]
    ex = [ph.sb([128, NE], F32, "ex") for _ in range(2)]
    gcl = [ph.sb([128, TB], F32, "gcl") for _ in range(2)]
    sg = [ph.sb([128, TB], F32, "sg") for _ in range(2)]
    l1 = [ph.sb([128, TB], F32, "l1") for _ in range(2)]
    tt1 = [ph.sb([128, TB], F32, "tt1") for _ in range(2)]
    bG = [ph.ps() for _ in range(2)]
    bL = [ph.ps() for _ in range(2)]
    bW = [ph.ps() for _ in range(1)]
    bD = [ph.ps() for _ in range(2)]
    bX = [ph.ps() for _ in range(1)]

    ph.load("pool", wr_t[:], I["w_router"][0].rearrange("(c p) e -> p c e", p=128), "c1", [wr_r])
    ph.load("sp", br_t[:], I["b_router"].partition_broadcast(128), "c2", [br_r])
    ph.load("sp", bg_t[:], I["b_gateT"][:, :, :], "c3", [bg_r])
    ph.load("sp", bl_t[:], I["b_linT"][:, :, :], "c4", [bl_r])
    ph.ts("dve", bl_t[:], bl_t[:], 1.0, None, ALU.add, None, [bl_r], [bl_r])
    ph.load("sp", bd_t[:], I["b_down"][0, :, :], "c5", [bd_r])
    ph.load("sp", c_t[:], I["c_csel"][:, :], "c6", [c_r])
    ph.load("sp", idf_t[:], I["c_ident"][:, :], "c7", [idf_r])

    rn = [0]

    def wload(src):
        slot = rn[0] % 4
        rn[0] += 1
        t, r = ring[slot]
        ph.load("pool", t[:], src.rearrange("(c p) f -> p c f", p=128), f"r{slot}", [r])
        return t, r

    k = 0
    dn = 0
    for tb in range(n_tb):
        tsl = slice(tb * TB, (tb + 1) * TB)
        ph.load("sp", xT_t[:], T["X1TS"][:, :, tsl].rearrange("c p t -> p c t"), "x1T", [xT_r])
        for tc in range(4):
            lgt, lgr = lg[tc % 2]
            t8t, t8r = t8[tc % 2]
            smt, smr = sm[tc % 2]
            mkt, mkr = mk[tc % 2]
            ext, exr = ex[tc % 2]
            xb, xbr = bX[0]
            for dc in range(16):
                ph.mm(xb[:, 0:NE], xT_t[:, dc, tc * 128:(tc + 1) * 128], wr_t[:, dc, :], dc == 0, dc == 15, [xT_r, wr_r], [xbr])
            ph.tt("dve", lgt[:], xb[:, 0:NE], br_t[:], ALU.add, [xbr, br_r], [lgr])
            ph.sc.op("dve", (lambda h_, a=t8t, b_=lgt: h_.max(a[:], b_[:])), [lgr], [t8r])
            ph.ts("dve", mkt[:], lgt[:], t8t[:, 3:4], None, ALU.is_ge, None, [lgr, t8r], [mkr])
            ph.ts("dve", smt[:, 0:1], t8t[:, 0:1], -1.0, None, ALU.mult, None, [t8r], [smr])
            ph.act(ext[:], lgt[:], AF.Exp, [lgr, smr], [exr], bias=smt[:, 0:1])
            ph.tt("dve", ext[:], ext[:], mkt[:], ALU.mult, [exr, mkr], [exr])
            ph.sc.op("dve", (lambda h_, a=smt, b_=ext: h_.reduce_sum(a[:, 1:2], b_[:], mybir.AxisListType.X)), [exr], [smr])
            ph.sc.op("dve", (lambda h_, a=smt: h_.reciprocal(a[:, 2:3], a[:, 1:2])), [smr], [smr])
            ph.ts("dve", ext[:], ext[:], smt[:, 2:3], None, ALU.mult, None, [exr, smr], [exr])
            ph.tr(xb[0:NE, 128:256], ext[:], idf_t[:], [exr, idf_r], [xbr])
            ph.cp("dve", wT_t[:, tc * 128:(tc + 1) * 128], xb[0:NE, 128:256], [xbr], [wT_r])
        if "WT" in T:
            ph.store("sp", T["WT"][:, tsl], wT_t[:], "wto", [wT_r])
        for nch in range(16):
            dt_, dr = bD[dn % 2]
            dn += 1
            ph.mm(dt_[:], bd_t[:, nch * 128:(nch + 1) * 128], wT_t[:], True, True, [bd_r, wT_r], [dr])
            ph.cp("act", acc_t[:, nch, :], dt_[:], [dr], [acc_r])
        for e in range(n_e):
            wb, wbr = bW[0]
            ph.mm(wb[:], c_t[:, e * 128:(e + 1) * 128], wT_t[:], True, True, [c_r, wT_r], [wbr])
            for fb in range(4):
                fsl = slice(fb * 512, (fb + 1) * 512)
                wg, wgr = wload(I["w_gate"][0, e, :, fsl])
                wl, wlr = wload(I["w_lin"][0, e, :, fsl])
                for fcl in range(4):
                    fc = fb * 4 + fcl
                    gt_, gr_ = bG[k % 2]
                    lt_, lr_ = bL[k % 2]
                    gc, gcr = gcl[k % 2]
                    sgt, sgr = sg[k % 2]
                    l1t, l1r = l1[k % 2]
                    t1t, t1r = tt1[k % 2]
                    k += 1
                    for dc in range(16):
                        ph.mm(gt_[:], wg[:, dc, fcl * 128:(fcl + 1) * 128], xT_t[:, dc, :], dc == 0, dc == 15, [wgr, xT_r], [gr_])
                    for dc in range(16):
                        ph.mm(lt_[:], wl[:, dc, fcl * 128:(fcl + 1) * 128], xT_t[:, dc, :], dc == 0, dc == 15, [wlr, xT_r], [lr_])
                    ph.ts("dve", gc[:], gt_[:], bg_t[:, e, fc:fc + 1], 7.0, ALU.add, ALU.min, [gr_, bg_r], [gcr])
                    ph.act(sgt[:], gc[:], AF.Sigmoid, [gcr], [sgr], scale=1.702)
                    ph.ts("dve", l1t[:], lt_[:], bl_t[:, e, fc:fc + 1], 8.0, ALU.add, ALU.min, [lr_, bl_r], [l1r])
                    ph.tt("dve", t1t[:], gc[:], wb[:], ALU.mult, [gcr, wbr], [t1r])
                    ph.stt("dve", t1t[:], l1t[:], -6.0, t1t[:], ALU.max, ALU.mult, [l1r, t1r], [t1r])
                    ph.tt("dve", hT_t[:, fc, :], t1t[:], sgt[:], ALU.mult, [t1r, sgr], [hT_r])
            for nb in range(4):
                wd, wdr = wload(I["w_down"][0, e, :, nb * 512:(nb + 1) * 512])
                for ncl in range(4):
                    nch = nb * 4 + ncl
                    dt_, dr = bD[dn % 2]
                    dn += 1
                    for fc in range(16):
                        ph.mm(dt_[:], wd[:, fc, ncl * 128:(ncl + 1) * 128], hT_t[:, fc, :], fc == 0, fc == 15, [wdr, hT_r], [dr])
                    ph.tt("dve", acc_t[:, nch, :], acc_t[:, nch, :], dt_[:], ALU.add, [acc_r, dr], [acc_r])
        ph.store("sp", T["FT"][:, :, tsl].rearrange("c p t -> p c t"), acc_t[:], "fto", [acc_r])
    ph.close()


def phase5(nc, I, T, out_ap, n_tb=NTB):
    ph = Phase(nc, "p5")
    g_t, g_r = ph.sb([128, D], F32, "lng")
    b_t, b_r = ph.sb([128, D], F32, "lnb")
    idf_t, idf_r = ph.sb([128, 128], F32, "identf")
    eps_t, eps_r = ph.sb([128, 1], F32, "eps")
    ft = [ph.sb([128, 16, 128], F32, "ft") for _ in range(2)]
    xc = [ph.sb([128, D], F32, "x1") for _ in range(2)]
    h2 = [ph.sb([128, D], F32, "h2") for _ in range(2)]
    ob = [ph.sb([128, D], F32, "ob") for _ in range(2)]
    junk_t, junk_r = ph.sb([128, D], BF16, "junk")
    stt_ = [ph.sb([128, 8], F32, "st") for _ in range(2)]
    banks = [ph.ps() for _ in range(8)]
    ph.memset("dve", eps_t[:], LN_EPS, [eps_r])
    ph.load("sp", g_t[:], I["ln2_g"].partition_broadcast(128), "c1", [g_r])
    ph.load("sp", b_t[:], I["ln2_b"].partition_broadcast(128), "c2", [b_r])
    ph.load("sp", idf_t[:], I["c_ident"][:, :], "c3", [idf_r])
    for n in range(n_tb * 4):
        r0 = n * 128
        ftt, ftr = ft[n % 2]
        xct, xcr = xc[n % 2]
        h2t, h2r = h2[n % 2]
        obt, obr = ob[n % 2]
        ph.load("sp", ftt[:], T["FT"][:, :, r0:r0 + 128].rearrange("c p t -> p c t"), f"ft{n % 2}", [ftr])
        ph.load("sp", xct[:], T["X1S"][r0:r0 + 128, :], f"x{n % 2}", [xcr])
        for q in range(4):
            bt, br = banks[(n % 2) * 4 + q]
            for kq in range(4):
                nch = q * 4 + kq
                ph.tr(bt[:, kq * 128:(kq + 1) * 128], ftt[:, nch, :], idf_t[:], [ftr, idf_r], [br])
            ph.stt("dve", h2t[:, q * 512:(q + 1) * 512], xct[:, q * 512:(q + 1) * 512], ALPHA, bt[:], ALU.mult, ALU.add, [xcr, br], [h2r])
        _layer_norm(ph, h2t, h2r, obt, obr, g_t, g_r, b_t, b_r, stt_[n % 2], eps_t, eps_r, junk_t, junk_r)
        ph.store("sp", out_ap[r0:r0 + 128, :], obt[:], f"o{n % 2}", [obr])
    ph.close()


def phaseS(nc, I, T, n_half, n_sel):
    ph = Phase(nc, "pS")
    ntc = n_half * 4
    sel_t, sel_r = ph.sb([128, ntc, 128], F32, "sel")
    id_t, id_r = ph.sb([128, 128], BF16, "ident")
    xc = [ph.sb([128, D], F32, "x") for _ in range(3)]
    xs = [ph.sb([128, D], F32, "xs") for _ in range(2)]
    xb = [ph.sb([128, D], BF16, "xb") for _ in range(2)]
    xT = [ph.sb([128, 16, TB], BF16, "x1T") for _ in range(2)]
    banks = [ph.ps() for _ in range(4)]
    trb = [ph.ps([128, 512], BF16, "tr") for _ in range(2)]
    ph.load("pool", id_t[:], I["c_ident"][:, :], "c", [id_r])
    xn = 0
    trn = 0
    for jc in range(n_sel * 4):
        ph.load("sp", sel_t[:], I["sel"][:, jc * 128:(jc + 1) * 128].rearrange("(c p) j -> p c j", p=128), "sel", [sel_r])
        xst, xsr = xs[jc % 2]
        xbt, xbr = xb[jc % 2]
        xTt, xTr = xT[(jc // 4) % 2]
        for tcn in range(ntc):
            xct, xcr = xc[xn % 3]
            ph.load("sp", xct[:], T["X1"][tcn * 128:(tcn + 1) * 128, :], f"x{xn % 3}", [xcr])
            xn += 1
            for nb in range(4):
                bt, br = banks[nb]
                ph.mm(bt[:], sel_t[:, tcn, :], xct[:, nb * 512:(nb + 1) * 512], tcn == 0, tcn == ntc - 1, [sel_r, xcr], [br])
        for nb in range(4):
            bt, br = banks[nb]
            ph.cp("dve" if nb % 2 else "act", xst[:, nb * 512:(nb + 1) * 512], bt[:], [br], [xsr])
        ph.store("sp", T["X1S"][jc * 128:(jc + 1) * 128, :], xst[:], f"xs{jc % 2}", [xsr])
        ph.cp("act", xbt[:], xst[:], [xsr], [xbr])
        tc = jc % 4
        for q in range(4):
            tt_, ttr = trb[trn % 2]
            trn += 1
            for k in range(4):
                dc = q * 4 + k
                ph.tr(tt_[:, k * 128:(k + 1) * 128], xbt[:, dc * 128:(dc + 1) * 128], id_t[:], [xbr, id_r], [ttr])
            ph.cp("dve" if q % 2 else "act", xTt[:, q * 4:(q + 1) * 4, tc * 128:(tc + 1) * 128],
                  tt_[:].rearrange("p (k t) -> p k t", k=4), [ttr], [xTr])
        if tc == 3:
            tb = jc // 4
            ph.store("sp", T["X1TS"][:, :, tb * TB:(tb + 1) * TB].rearrange("c p t -> p c t"), xTt[:], f"xT{tb % 2}", [xTr])
    ph.close()


ALL_PHASES = ("0", "1", "2a", "2b", "3a", "3b", "S", "4", "5")


N_DIR = 2
N_PER = 4
N_CORES = N_DIR * N_PER


def kernel(**inputs):
    half_tb = NTB // N_DIR
    sel_tb = half_tb // N_PER
    nc = build(phases=ALL_PHASES, n_tb=half_tb, n_sel=sel_tb)
    maps = []
    for c in range(N_CORES):
        maps.append(host_inputs(inputs, rev=(c // N_PER == 1), n_half=half_tb, n_sel=sel_tb, q=c % N_PER))
    res = run_bass_kernel_spmd(nc, maps, core_ids=list(range(N_CORES)))
    parts = []
    for c in range(N_PER):
        parts.append(np.asarray(res.results[c]["out"], dtype=np.float32))
    for c in reversed(range(N_PER)):
        parts.append(np.asarray(res.results[N_PER + c]["out"], dtype=np.float32)[::-1])
    out = np.concatenate(parts, axis=0)
    return out.reshape(1, S, D)
```

```python
import contextlib
import math
import numpy as np
import concourse.bass as bass
import concourse.mybir as mybir
from concourse.bass_utils import run_bass_kernel_spmd

F32 = mybir.dt.float32
BF16 = mybir.dt.bfloat16
AF = mybir.ActivationFunctionType
ALU = mybir.AluOpType

S = 8192
D = 2048
HD = 128
IN_W = 10240
NE = 32
TB = 512
NTB = S // TB
A_GROUPS = ((128, 1), (512, 4), (2048, 16))
ALPHA = 2.0 ** 0.25
LN_EPS = 1e-5
QK_EPS = 1e-6
NEG = -30000.0


class Res:
    __slots__ = ("name", "w", "rd")

    def __init__(self, name):
        self.name = name
        self.w = None
        self.rd = {}


class Ev:
    __slots__ = ("kind", "eng", "seq", "marked", "sem", "val")

    def __init__(self, kind, eng, seq):
        self.kind = kind
        self.eng = eng
        self.seq = seq
        self.marked = False
        self.sem = None
        self.val = None


class Ins:
    __slots__ = ("fn", "waits", "ev")


ENGS = ("pe", "act", "dve", "pool", "sp")
EPOCH = 30000


class Sched:
    def __init__(self):
        self.streams = {e: [] for e in ENGS}
        self.waited = {e: {} for e in ENGS}
        self.dcount = {}

    def _key(self, ev):
        return ev.eng

    def _add(self, eng, fn, reads, writes, ev):
        ins = Ins()
        ins.fn = fn
        ins.ev = ev
        ins.waits = []
        deps = []
        for r in reads:
            if r.w is not None:
                deps.append(r.w)
        for w in writes:
            if w.w is not None:
                deps.append(w.w)
            deps.extend(w.rd.values())
        wd = self.waited[eng]
        for d in deps:
            if d is ev:
                continue
            if d.kind == "c" and d.eng == "pe" and eng == "pe":
                continue
            k = d.eng
            if wd.get(k, -1) >= d.seq:
                continue
            wd[k] = d.seq
            d.marked = True
            ins.waits.append(d)
        for r in reads:
            r.rd[ev.eng] = ev
        for w in writes:
            w.w = ev
            w.rd = {}
        self.streams[eng].append(ins)
        return ins

    def op(self, eng, fn, reads=(), writes=()):
        ev = Ev("c", eng, len(self.streams[eng]))
        self._add(eng, fn, reads, writes, ev)

    def dma(self, queue, fn, dsem, reads=(), writes=()):
        c = self.dcount.get(dsem, 0) + 1
        self.dcount[dsem] = c
        ev = Ev("d", dsem, c)
        ev.marked = True
        self._add(queue, fn, reads, writes, ev)
        return ev

    def emit(self, nc, final_events, name=""):
        sems = nc._k_sem_stack
        cst = nc._k_csem
        dst = nc._k_dsem
        with contextlib.ExitStack() as st:
            for e in ENGS:
                for ins in self.streams[e]:
                    ev = ins.ev
                    if ev.kind == "c" and ev.marked:
                        cur = cst.get(e)
                        if cur is None or cur[1] >= EPOCH:
                            nsem = sems.enter_context(nc.semaphore(f"c_{e}_{len(sems._exit_callbacks)}"))
                            cur = [nsem, 0]
                            cst[e] = cur
                        cur[1] += 1
                        ev.sem = cur[0]
                        ev.val = cur[1]
            base = {}
            for k in self.dcount:
                if k not in dst:
                    dst[k] = [sems.enter_context(nc.semaphore(f"d_{k}")), 0]
                base[k] = dst[k][1]
            for e in ENGS:
                for ins in self.streams[e]:
                    if ins.ev.kind == "d":
                        ins.ev.sem = dst[ins.ev.eng][0]
                        ins.ev.val = 16 * (base[ins.ev.eng] + ins.ev.seq)
            for k, c in self.dcount.items():
                dst[k][1] = base[k] + c
            block = st.enter_context(nc.Block())

            def run(e):
                def body(h):
                    for ins in self.streams[e]:
                        for d in ins.waits:
                            h.wait_ge(d.sem, d.val)
                        bi = ins.fn(h)
                        ev = ins.ev
                        if ev.kind == "d":
                            bi.then_inc(ev.sem, 16)
                        elif ev.marked:
                            bi.then_inc(ev.sem, 1)
                    for d in final_events:
                        h.wait_ge(d.sem, d.val)
                return body

            block.tensor(run("pe"))
            block.scalar(run("act"))
            block.vector(run("dve"))
            block.gpsimd(run("pool"))
            block.sync(run("sp"))


def _t5_bucket(rel):
    nb = 16
    max_exact = 8
    n = np.abs(rel)
    nf = np.maximum(n, 1).astype(np.float32)
    large = max_exact + (np.log(nf / max_exact) / math.log(1024 / max_exact) * (nb - max_exact)).astype(np.int32)
    large = np.minimum(large, nb - 1)
    return np.where(rel > 0, nb, 0) + np.where(n < max_exact, n, large)


def _host_consts(rev=False):
    c = {}
    c["ident"] = np.eye(128, dtype=np.float32)
    c["ones"] = np.ones((128, 128), np.float32)
    pt = np.zeros((128, 128), np.float32)
    for base in (0, 64):
        for i in range(32):
            pt[base + i + 32, base + i] = -1.0
            pt[base + i, base + i + 32] = 1.0
    c["ropeP"] = pt
    t = np.arange(S)
    if rev:
        t = t[::-1]
    row = (t // 64).astype(np.float32)
    col = (t % 64).astype(np.float32)
    inv = (10000.0 ** (-np.arange(0, 64, 2, dtype=np.float32) / 64)).astype(np.float32)
    ang_r = row[None, :] * inv[:, None]
    ang_c = col[None, :] * inv[:, None]
    cosT = np.concatenate([np.cos(ang_r), np.cos(ang_r), np.cos(ang_c), np.cos(ang_c)], 0)
    sinT = np.concatenate([np.sin(ang_r), np.sin(ang_r), np.sin(ang_c), np.sin(ang_c)], 0)
    ohv = np.zeros((33, 3, 384), np.float32)
    for gi, (_, dil) in enumerate(A_GROUPS):
        for xi in range(384):
            rel = xi - 127 - 64
            if xi < 383 and abs(rel) <= 64:
                ohv[int(_t5_bucket(np.array((-rel if rev else rel) * dil))), gi, xi] = 1.0
            else:
                ohv[32, gi, xi] = NEG
    c["ohv"] = ohv
    cs = np.zeros((NE, NE * 128), np.float32)
    for e in range(NE):
        cs[e, e * 128:(e + 1) * 128] = 1.0
    c["csel"] = cs
    c["antiI"] = np.ascontiguousarray(np.eye(128, dtype=np.float32)[::-1])
    eb = np.zeros((128, 3), np.float32)
    eb[:64, 1] = NEG
    eb[64:, 2] = NEG
    c["edge"] = eb
    c["cosT"] = cosT.astype(np.float32)
    c["sinT"] = sinT.astype(np.float32)
    return c


class Phase:
    def __init__(self, nc, name):
        self.nc = nc
        self.name = name
        self.sc = Sched()
        self.st = contextlib.ExitStack()
        self.n = 0
        self.finals = []

    CONST_TAGS = ("rb", "oh", "ones", "ropeP", "gqk", "bg", "eps", "tab", "antiI", "edge", "wa", "wb", "wo", "lng",
                  "lnb", "ident", "wr", "br", "bl", "bd", "csel", "identf")

    def sb(self, shape, dt, tag="t"):
        self.n += 1
        t = self.st.enter_context(self.nc.sbuf_tensor(f"{self.name}_{tag}{self.n}", list(shape), dt))
        if tag in self.CONST_TAGS:
            if not hasattr(self, "cres"):
                self.cres = Res("const")
            return t, self.cres
        return t, Res(f"{tag}{self.n}")

    def ps(self, shape=(128, 512), dt=F32, tag="ps"):
        self.n += 1
        t = self.st.enter_context(self.nc.psum_tensor(f"{self.name}_{tag}{self.n}", list(shape), dt))
        return t, Res(f"{tag}{self.n}")

    def mm(self, out, lhsT, rhs, start, stop, reads, writes):
        self.sc.op("pe", lambda h: h.matmul(out, lhsT, rhs, start=start, stop=stop), reads, writes)

    def tr(self, out, in_, ident, reads, writes):
        self.sc.op("pe", lambda h: h.transpose(out, in_, ident), reads, writes)

    def act(self, out, in_, func, reads, writes, bias=None, scale=None, accum_out=None):
        kw = {}
        if bias is not None:
            kw["bias"] = bias
        if scale is not None:
            kw["scale"] = scale
        if accum_out is not None:
            kw["accum_out"] = accum_out
        self.sc.op("act", lambda h: h.activation(out, in_, func, **kw), reads, writes)

    def cp(self, eng, out, in_, reads, writes):
        if eng == "act":
            self.sc.op("act", lambda h: h.copy(out, in_), reads, writes)
        else:
            self.sc.op(eng, lambda h: h.tensor_copy(out, in_), reads, writes)

    def ts(self, eng, out, in0, s1, s2, op0, op1, reads, writes, accum_out=None):
        if op1 is None:
            self.sc.op(eng, lambda h: h.tensor_scalar(out, in0, s1, None, op0), reads, writes)
        elif accum_out is not None:
            self.sc.op(eng, lambda h: h.tensor_scalar(out, in0, s1, s2, op0, op1, accum_out), reads, writes)
        else:
            self.sc.op(eng, lambda h: h.tensor_scalar(out, in0, s1, s2, op0, op1), reads, writes)

    def tt(self, eng, out, in0, in1, op, reads, writes):
        self.sc.op(eng, lambda h: h.tensor_tensor(out, in0, in1, op), reads, writes)

    def stt(self, eng, out, in0, scalar, in1, op0, op1, reads, writes):
        self.sc.op(eng, lambda h: h.scalar_tensor_tensor(out, in0, scalar, in1, op0, op1), reads, writes)

    def memset(self, eng, ap, val, writes):
        self.sc.op(eng, lambda h: h.memset(ap, val), (), writes)

    def load(self, queue, out, in_, dsem, writes, reads=()):
        if writes and writes[0] is getattr(self, "cres", None):
            dsem = "c"
        return self.sc.dma(queue, lambda h: h.dma_start(out=out, in_=in_), dsem, reads, writes)

    def store(self, queue, out, in_, dsem, reads, writes=()):
        ev = self.sc.dma(queue, lambda h: h.dma_start(out=out, in_=in_), dsem, reads, writes)
        self.finals.append(ev)
        return ev

    def close(self):
        last = {}
        for e in ENGS:
            for ins in self.sc.streams[e]:
                last[ins.ev.eng] = ins.ev
        for ev in last.values():
            ev.marked = True
        self.sc.emit(self.nc, list(last.values()), self.name)
        self.st.close()


def _dram(nc, name, shape, dt, kind):
    return nc.dram_tensor(name, list(shape), dt, kind=kind).ap()


def phase1(nc, I, T, n_tb=NTB):
    ph = Phase(nc, "p1")
    wbl = [ph.sb([128, 16, 512], BF16, "w") for _ in range(2)]
    xbl = [ph.sb([128, 16, 512], BF16, "x") for _ in range(3)]
    obl = [ph.sb([128, 4, 512], BF16, "o") for _ in range(2)]
    banks = [ph.ps() for _ in range(8)]
    ones_t, ones_r = ph.sb([128, 128], BF16, "ones")
    rp_t, rp_r = ph.sb([128, 128], BF16, "ropeP")
    gq_t, gq_r = ph.sb([128, 2], F32, "gqk")
    bg_t, bg_r = ph.sb([128, 32], F32, "bg")
    cs = [ph.sb([128, 2, 512], F32, "cs") for _ in range(2)]
    sq = [ph.sb([128, 512], BF16, "sq") for _ in range(2)]
    r0 = [ph.sb([128, 512], F32, "r0") for _ in range(2)]
    kn = [ph.sb([128, 512], BF16, "kn") for _ in range(2)]
    ta = [ph.sb([128, 512], F32, "ta") for _ in range(2)]
    tb_ = [ph.sb([128, 512], F32, "tb") for _ in range(2)]

    eps_t, eps_r = ph.sb([128, 1], F32, "eps")
    ph.memset("dve", eps_t[:], 128.0 * QK_EPS, [eps_r])
    ph.load("pool", ones_t[:], I["c_ones"][:, :], "c1", [ones_r])
    ph.load("pool", rp_t[:], I["c_ropeP"][:, :], "c2", [rp_r])
    ph.load("sp", gq_t[:], I["qk_normT"][:, :], "c3", [gq_r])
    ph.load("sp", bg_t[:], I["b_gatesT"][:, :], "c4", [bg_r])
    ph.ts("dve", gq_t[:], gq_t[:], math.sqrt(128.0), None, ALU.mult, None, [gq_r], [gq_r])

    bank_i = [0]
    xcnt = [0]
    ocnt = [0]
    rcnt = [0]
    cpe = [0]

    def next_bank():
        b = banks[bank_i[0] % 8]
        bank_i[0] += 1
        return b

    def fm_chunk(wt, wr, xt, xr, j):
        pt, pr = next_bank()
        for dc in range(16):
            ph.mm(pt[:], wt[:, dc, j * 128:(j + 1) * 128], xt[:, dc, :], dc == 0, dc == 15, [wr, xr], [pr])
        return pt, pr

    def rope_epilogue(pt, pr, gcol, cst, csr, out_ap, out_r):
        i = rcnt[0] % 2
        rcnt[0] += 1
        sq_t, sq_r = sq[i]
        r0_t, r0_r = r0[i]
        kn_t, kn_r = kn[i]
        ta_t, ta_r = ta[i]
        tb_t, tb_r = tb_[i]
        ph.act(sq_t[:], pt[:], AF.Square, [pr], [sq_r])
        p2, p2r = next_bank()
        ph.mm(p2[:], ones_t[:], sq_t[:], True, True, [ones_r, sq_r], [p2r])
        ph.act(tb_t[:], p2[:], AF.Sqrt, [p2r, eps_r], [tb_r], bias=eps_t[:, 0:1], scale=1.0)
        ph.sc.op("dve", (lambda h_, a=r0_t, b=tb_t: h_.reciprocal(a[:], b[:])), [tb_r], [r0_r])
        ph.stt("dve", kn_t[:], pt[:], gq_t[:, gcol:gcol + 1], r0_t[:], ALU.mult, ALU.mult, [pr, gq_r, r0_r], [kn_r])
        p3, p3r = next_bank()
        ph.mm(p3[:], rp_t[:], kn_t[:], True, True, [rp_r, kn_r], [p3r])
        ph.tt("dve", ta_t[:], kn_t[:], cst[:, 0, :], ALU.mult, [kn_r, csr], [ta_r])
        ph.tt("dve", tb_t[:], p3[:], cst[:, 1, :], ALU.mult, [p3r, csr], [tb_r])
        ph.tt("dve", out_ap, ta_t[:], tb_t[:], ALU.add, [ta_r, tb_r], [out_r])

    for g in range(20):
        wt, wr = wbl[g % 2]
        ph.load("pool", wt[:], I["w_in"][0, :, g * 512:(g + 1) * 512].rearrange("(c p) f -> p c f", p=128), f"w{g % 2}", [wr])
        own_only = g < 3 or g in (9, 10) or g >= 12
        n_blk = n_tb if own_only else (min(NTB, n_tb + 2) if 3 <= g <= 8 else NTB)
        for tb in range(n_blk):
            xs = xcnt[0] % 3
            xcnt[0] += 1
            xt, xr = xbl[xs]
            ph.load("pool", xt[:], I["xT"][:, tb * TB:(tb + 1) * TB].rearrange("(c p) t -> p c t", p=128), f"x{xs}", [xr])
            os_ = ocnt[0] % 2
            ocnt[0] += 1
            ot, orr = obl[os_]
            tsl = slice(tb * TB, (tb + 1) * TB)
            need_cs = g in (9, 10, 11)
            if need_cs:
                cst, csr = cs[tb % 2]
                ph.load("sp", cst[:, 0, :], I["c_cosT"][:, tsl], f"cs{tb % 2}", [csr])
                ph.load("sp", cst[:, 1, :], I["c_sinT"][:, tsl], f"cs{tb % 2}", [csr])
            if g < 6 or g >= 12:
                for j in range(4):
                    pt, pr = fm_chunk(wt, wr, xt, xr, j)
                    if g >= 12:
                        fc = (g - 12) * 4 + j
                        ph.act(ot[:, j, :], pt[:], AF.Sigmoid, [pr, bg_r], [orr], bias=bg_t[:, fc:fc + 1])
                    else:
                        eng = "act" if cpe[0] % 2 == 0 else "dve"
                        cpe[0] += 1
                        ph.cp(eng, ot[:, j, :], pt[:], [pr], [orr])
                if g < 3:
                    dst = T["QAT"][g * 4:(g + 1) * 4, :, tsl]
                elif g < 6:
                    dst = T["KAT"][(g - 3) * 4:(g - 2) * 4, :, tsl]
                else:
                    dst = T["GT"][(g - 12) * 4:(g - 11) * 4, :, tsl]
                ph.store("sp", dst.rearrange("c p t -> p c t"), ot[:], f"o{os_}", [orr])
            elif g in (9, 10):
                for j in range(4):
                    pt, pr = fm_chunk(wt, wr, xt, xr, j)
                    rope_epilogue(pt, pr, 0, cst, csr, ot[:, j, :], orr)
                dst = T["QBT"][(g - 9) * 4:(g - 8) * 4, :, tsl]
                ph.store("sp", dst.rearrange("c p t -> p c t"), ot[:], f"o{os_}", [orr])
            elif g in (6, 7, 8):
                for tc in range(4):
                    pt, pr = next_bank()
                    for dc in range(16):
                        ph.mm(pt[:], xt[:, dc, tc * 128:(tc + 1) * 128], wt[:, dc, :], dc == 0, dc == 15, [wr, xr], [pr])
                    eng = "act" if cpe[0] % 2 == 0 else "dve"
                    cpe[0] += 1
                    ph.cp(eng, ot[:, tc, :], pt[:], [pr], [orr])
                dst = T["VA"][tsl, (g - 6) * 512:(g - 5) * 512]
                ph.store("sp", dst.rearrange("(c p) f -> p c f", p=128), ot[:], f"o{os_}", [orr])
            else:
                for j in range(2):
                    pt, pr = fm_chunk(wt, wr, xt, xr, j)
                    rope_epilogue(pt, pr, 1, cst, csr, ot[:, j, :], orr)
                dst = T["KBT"][0:2, :, tsl]
                ph.store("sp", dst.rearrange("c p t -> p c t"), ot[:, 0:2, :], f"o{os_}", [orr])
                os2 = ocnt[0] % 2
                ocnt[0] += 1
                ot2, orr2 = obl[os2]
                for tc in range(4):
                    pt, pr = next_bank()
                    for dc in range(16):
                        ph.mm(pt[:, 0:256], xt[:, dc, tc * 128:(tc + 1) * 128], wt[:, dc, 256:512], dc == 0, dc == 15, [wr, xr], [pr])
                    ph.cp("act", ot2[:, tc, 0:256], pt[:, 0:256], [pr], [orr2])
                dst = T["VB"][tsl, :]
                ph.store("sp", dst.rearrange("(c p) f -> p c f", p=128), ot2[:, :, 0:256], f"o{os2}", [orr2])
    ph.close()


def phase2b(nc, I, T, n_tb=NTB):
    ph = Phase(nc, "p2b")
    kt_t, kt_r = ph.sb([128, S], BF16, "kT")
    v_t, v_r = ph.sb([128, 64, 128], BF16, "v")
    ones_t, ones_r = ph.sb([128, 128], BF16, "ones")
    qb = [ph.sb([128, TB], BF16, "q") for _ in range(2)]
    pb = [ph.sb([128, TB], BF16, "p") for _ in range(3)]
    ob = [ph.sb([128, TB], BF16, "o") for _ in range(2)]
    rc = [ph.sb([128, TB], F32, "rc") for _ in range(2)]
    sbank = [ph.ps() for _ in range(3)]
    obank = [ph.ps() for _ in range(2)]
    dbank = [ph.ps() for _ in range(2)]
    ph.load("pool", ones_t[:], I["c_ones"][:, :], "c1", [ones_r])
    scale = 1.0 / math.sqrt(HD)
    u = 0
    pc = 0
    for kvh in range(2):
        ph.load("sp", kt_t[:], T["KBT"][kvh, :, :], "kt", [kt_r])
        ph.load("sp", v_t[:], T["VB"][:, kvh * 128:(kvh + 1) * 128].rearrange("(c p) f -> p c f", p=128), "v", [v_r])
        for hh in range(4):
            h = kvh * 4 + hh
            for qi in range(n_tb):
                qt, qr = qb[u % 2]
                ot, orr = ob[u % 2]
                rt, rr = rc[u % 2]
                op_, opr = obank[u % 2]
                dp, dpr = dbank[u % 2]
                tsl = slice(qi * TB, (qi + 1) * TB)
                ph.load("sp", qt[:], T["QBT"][h, :, tsl], f"q{u % 2}", [qr])
                for kc in range(64):
                    sp_, spr = sbank[pc % 3]
                    pt, pr = pb[pc % 3]
                    pc += 1
                    ph.mm(sp_[:], kt_t[:, kc * 128:(kc + 1) * 128], qt[:], True, True, [kt_r, qr], [spr])
                    ph.act(pt[:], sp_[:], AF.Exp, [spr], [pr], scale=scale)
                    ph.mm(op_[:], v_t[:, kc, :], pt[:], kc == 0, kc == 63, [v_r, pr], [opr])
                    ph.mm(dp[:], ones_t[:], pt[:], kc == 0, kc == 63, [ones_r, pr], [dpr])
                ph.sc.op("dve", (lambda h_, a=rt, b=dp: h_.reciprocal(a[:], b[:])), [dpr], [rr])
                ph.tt("dve", ot[:], op_[:], rt[:], ALU.mult, [opr, rr], [orr])
                ph.store("sp", T["YBT"][h, :, tsl], ot[:], f"o{u % 2}", [orr])
                u += 1
    ph.close()


SCRATCH = {
    "QAT": ([12, 128, S], BF16), "KAT": ([12, 128, S], BF16), "VA": ([S, 1536], BF16),
    "QBT": ([8, 128, S], BF16), "KBT": ([2, 128, S], BF16), "VB": ([S, 256], BF16),
    "GT": ([32, 128, S], BF16), "YBT": ([8, 128, S], BF16), "YAT": ([4, 128, S], BF16),
    "X1S": ([S, D], F32), "X1TS": ([16, 128, S], BF16), "U": ([12, 512], F32), "YT": ([16, 128, S], BF16), "FT": ([16, 128, S], F32), "X1": ([S, D], F32), "X1T": ([16, 128, S], BF16),
}


def build(phases=("1", "2b"), dbg=(), n_tb=NTB, n_e=NE, n_sel=None):
    if n_sel is None:
        n_sel = n_tb
    nc = bass.Bass("TRN2", target_bir_lowering=False)
    nc._k_sem_stack = contextlib.ExitStack()
    nc._k_csem = {}
    nc._k_dsem = {}
    I = {}
    hc = _host_consts()

    def din(name, shape):
        I[name] = _dram(nc, name, shape, F32, "ExternalInput")

    din("xT", [D, S])
    din("x", [S, D])
    din("w_in", [1, D, IN_W])
    din("qk_normT", [128, 2])
    din("b_gatesT", [128, 32])
    din("rel_bias", [32, 12])
    din("w_branch_a", [1, 512, D])
    din("w_branch_b", [1, 1024, D])
    din("w_out", [1, D, D])
    for nm in ("ln1_g", "ln1_b", "ln2_g", "ln2_b"):
        din(nm, [1, D])
    din("w_router", [1, D, NE])
    din("b_router", [1, NE])
    din("b_gateT", [128, NE, 16])
    din("b_linT", [128, NE, 16])
    din("b_down", [1, NE, D])
    for nm in ("w_gate", "w_lin", "w_down"):
        din(nm, [1, n_e, D, D])
    out_ap = _dram(nc, "out", [n_sel * TB, D], F32, "ExternalOutput")
    din("sel", [n_tb * TB, n_sel * TB])
    for k, v in hc.items():
        din("c_" + k, list(v.shape))
    T = {}
    for k, (shape, dt) in SCRATCH.items():
        T[k] = _dram(nc, "s_" + k, shape, dt, "ExternalOutput" if k in dbg else "Internal")
    if "0" in phases:
        phase0(nc, I, T)
    if "1" in phases:
        phase1(nc, I, T, n_tb)
    if "2a" in phases:
        phase2a(nc, I, T, n_tb)
    if "2b" in phases:
        phase2b(nc, I, T, n_tb)
    if "3a" in phases:
        phase3a(nc, I, T, n_tb)
    if "3b" in phases:
        phase3b(nc, I, T, n_tb)
    if "WT" in dbg:
        T["WT"] = _dram(nc, "s_WT", [NE, S], F32, "ExternalOutput")
    if "S" in phases:
        phaseS(nc, I, T, n_tb, n_sel)
    if "4" in phases:
        phase4(nc, I, T, n_sel, n_e)
    if "5" in phases:
        phase5(nc, I, T, out_ap, n_sel)
    nc._k_sem_stack.close()
    return nc


def host_inputs(inp, n_e=NE, rev=False, n_half=NTB, n_sel=None, q=0):
    m = {}
    x = np.asarray(inp["x"], np.float32)[0]
    if rev:
        x = x[::-1]
    m["x"] = np.ascontiguousarray(x)
    m["xT"] = np.ascontiguousarray(x.T)
    m["w_in"] = np.asarray(inp["w_in"], np.float32)
    m["qk_normT"] = np.ascontiguousarray(np.stack([np.asarray(inp["q_norm"])[0], np.asarray(inp["k_norm"])[0]], 1).astype(np.float32))
    m["b_gatesT"] = np.ascontiguousarray(np.asarray(inp["b_gates"], np.float32)[0].reshape(32, 128).T)
    for nm in ("w_branch_a", "w_branch_b", "w_out", "ln1_g", "ln1_b", "ln2_g", "ln2_b"):
        m[nm] = np.ascontiguousarray(np.asarray(inp[nm], np.float32))
    for nm in ("w_router", "b_router", "b_down"):
        m[nm] = np.asarray(inp[nm], np.float32)
    for nm in ("w_gate", "w_lin", "w_down"):
        m[nm] = np.asarray(inp[nm], np.float32)[:, :n_e]
    m["b_gateT"] = np.ascontiguousarray(np.asarray(inp["b_gate"], np.float32)[0].reshape(NE, 16, 128).transpose(2, 0, 1))
    m["b_linT"] = np.ascontiguousarray(np.asarray(inp["b_lin"], np.float32)[0].reshape(NE, 16, 128).transpose(2, 0, 1))
    m["rel_bias"] = np.ascontiguousarray(np.asarray(inp["rel_bias"], np.float32))
    for k, v in _host_consts(rev).items():
        m["c_" + k] = v
    if n_sel is None:
        n_sel = n_half
    sel = np.zeros((n_half * TB, n_sel * TB), np.float32)
    j = np.arange(n_sel * TB)
    sel[q * n_sel * TB + j, j] = 1.0
    m["sel"] = sel
    return m


def phase0(nc, I, T):
    ph = Phase(nc, "p0")
    rb_t, rb_r = ph.sb([33, 12], F32, "rb")
    oh_t, oh_r = ph.sb([33, 3, 384], F32, "oh")
    u_t, u_r = ph.sb([4, 3, 384], F32, "u")
    ph.memset("dve", rb_t[32:33, :], 1.0, [rb_r])
    ph.load("sp", rb_t[0:32, :], I["rel_bias"][:, :], "c1", [rb_r])
    ph.load("sp", oh_t[:], I["c_ohv"][:, :, :], "c2", [oh_r])
    for g in range(3):
        pt, pr = ph.ps([4, 384])
        ph.mm(pt[:], rb_t[:, 4 * g:4 * g + 4], oh_t[:, g, :], True, True, [rb_r, oh_r], [pr])
        ph.cp("dve", u_t[:, g, :], pt[:], [pr], [u_r])
        ph.store("sp", T["U"][4 * g:4 * g + 4, 0:384], u_t[:, g, :], f"o{g}", [u_r])
    ph.close()


def phase2a(nc, I, T, n_tb=NTB):
    ph = Phase(nc, "p2a")
    oacc_t, oacc_r = ph.sb([128, S], F32, "oacc")
    dacc_t, dacc_r = ph.sb([128, S], F32, "dacc")
    tab_t, tab_r = ph.sb([128, 12, 2, 128], F32, "tab")
    hk = [ph.sb([128, 128], F32, "hk") for _ in range(2)]
    aI_t, aI_r = ph.sb([128, 128], F32, "antiI")
    ones_t, ones_r = ph.sb([128, 128], BF16, "ones")
    eb_t, eb_r = ph.sb([128, 3], F32, "edge")
    kw = [ph.sb([128, 4096], BF16, "kw") for _ in range(2)]
    qw = [ph.sb([128, 2048], BF16, "qw") for _ in range(2)]
    vw = [ph.sb([128, 16, 2, 128], BF16, "vw") for _ in range(2)]
    tmp = [ph.sb([128, 128], F32, "tmp") for _ in range(3)]
    pb = [ph.sb([128, 128], BF16, "p") for _ in range(3)]
    rc_t, rc_r = ph.sb([128, 2048], F32, "rc")
    yb = [ph.sb([128, 2048], BF16, "y") for _ in range(2)]
    sbank = [ph.ps([128, 128]) for _ in range(3)]
    obank = [ph.ps([128, 128]) for _ in range(2)]
    dbank = [ph.ps([128, 128]) for _ in range(2)]
    tbank = ph.ps([128, 128])

    ph.load("sp", aI_t[:], I["c_antiI"][:, :], "c1", [aI_r])
    ph.load("pool", ones_t[:], I["c_ones"][:, :], "c2", [ones_r])
    ph.load("sp", eb_t[:], I["c_edge"][:, :], "c3", [eb_r])
    for i in range(2):
        ph.memset("pool", kw[i][0][:], 0.0, [kw[i][1]])
        ph.memset("pool", vw[i][0][:], 0.0, [vw[i][1]])
    n = 0
    for hd_ in range(12):
        for c in range(2):
            ht, hr = hk[n % 2]
            n += 1
            src = bass.AP(T["U"].tensor, hd_ * 512 + c * 128, [[1, 128], [1, 128]])
            ph.load("sp", ht[:], src, f"hk{n % 2}", [hr])
            ph.mm(tbank[0][:], ht[:], aI_t[:], True, True, [hr, aI_r], [tbank[1]])
            ph.cp("dve", tab_t[:, hd_, c, :], tbank[0][:], [tbank[1]], [tab_r])

    scale = 1.0 / math.sqrt(HD)
    wn = 0
    pc = 0
    un = 0
    for j in range(4):
        for g, (_, d) in enumerate(A_GROUPS):
            head = 4 * g + j
            span = 128 * d
            nb = S // span
            var = T["VA"].rearrange("(n d) f -> d n f", d=d)
            for b in range(nb * n_tb // NTB):
                base = b * span
                kt, kr = kw[wn % 2]
                qt, qr = qw[wn % 2]
                vt, vr = vw[wn % 2]
                wn += 1
                lo = base - 64 * d
                hi = base + 192 * d
                clo, chi = max(lo, 0), min(hi, S)
                ph.load("sp", kt[:, clo - lo:chi - lo], T["KAT"][head, :, clo:chi], f"kw{wn % 2}", [kr])
                ph.load("sp", qt[:, 0:span], T["QAT"][head, :, base:base + span], f"qw{wn % 2}", [qr])
                n0 = base // d - 64
                for c in range(2):
                    p0, p1 = 0, 128
                    if n0 + c * 128 < 0:
                        p0 = -(n0 + c * 128)
                    if n0 + (c + 1) * 128 > S // d:
                        p1 = S // d - (n0 + c * 128)
                    src = var[:, n0 + c * 128 + p0:n0 + c * 128 + p1, head * 128:(head + 1) * 128].rearrange("r p f -> p r f")
                    ph.load("pool", vt[p0:p1, 0:d, c, :], src, f"vw{wn % 2}", [vr])
                for r in range(d):
                    op_, opr = obank[un % 2]
                    dp, dpr = dbank[un % 2]
                    un += 1
                    for c in range(2):
                        sp_, spr = sbank[pc % 3]
                        tt_, ttr = tmp[pc % 3]
                        pt, pr = pb[pc % 3]
                        pc += 1
                        k0 = r + c * 128 * d
                        ph.mm(sp_[:], kt[:, k0:k0 + 127 * d + 1:d], qt[:, r:r + 127 * d + 1:d], True, True, [kr, qr], [spr])
                        ph.stt("dve", tt_[:], sp_[:], scale, tab_t[:, head, c, :], ALU.mult, ALU.add, [spr, tab_r], [ttr])
                        ecol = 0
                        if b == 0 and c == 0:
                            ecol = 1
                        if b == nb - 1 and c == 1:
                            ecol = 2
                        ph.act(pt[:], tt_[:], AF.Exp, [ttr, eb_r], [pr], bias=eb_t[:, ecol:ecol + 1])
                        ph.mm(op_[:], vt[:, r, c, :], pt[:], c == 0, c == 1, [vr, pr], [opr])
                        ph.mm(dp[:], ones_t[:], pt[:], c == 0, c == 1, [ones_r, pr], [dpr])
                    osl = oacc_t[:, base + r:base + r + 127 * d + 1:d]
                    dsl = dacc_t[:, base + r:base + r + 127 * d + 1:d]
                    if g == 0:
                        ph.cp("dve", osl, op_[:], [opr], [oacc_r])
                        ph.cp("pool" if False else "dve", dsl, dp[:], [dpr], [dacc_r])
                    else:
                        ph.tt("dve", osl, osl, op_[:], ALU.add, [opr, oacc_r], [oacc_r])
                        ph.tt("dve", dsl, dsl, dp[:], ALU.add, [dpr, dacc_r], [dacc_r])
        for seg in range(4 * n_tb // NTB):
            yt, yr = yb[seg % 2]
            ssl = slice(seg * 2048, (seg + 1) * 2048)
            ph.sc.op("dve", (lambda h_, a=rc_t, b_=dacc_t, sl=ssl: h_.reciprocal(a[:], b_[:, sl])), [dacc_r], [rc_r])
            ph.tt("dve", yt[:], oacc_t[:, ssl], rc_t[:], ALU.mult, [oacc_r, rc_r], [yr])
            ph.store("sp", T["YAT"][j, :, ssl], yt[:], f"y{seg % 2}", [yr])
    ph.close()


def phase3a(nc, I, T, n_tb=NTB):
    ph = Phase(nc, "p3a")
    wa_t, wa_r = ph.sb([128, 4, D], BF16, "wa")
    wb_t, wb_r = ph.sb([128, 8, D], BF16, "wb")
    ya = [ph.sb([128, 4, TB], BF16, "ya") for _ in range(2)]
    yb = [ph.sb([128, 8, TB], BF16, "yb") for _ in range(2)]
    gt = [ph.sb([128, 2, TB], BF16, "g") for _ in range(3)]
    t1 = [ph.sb([128, TB], F32, "t1") for _ in range(2)]
    t2 = [ph.sb([128, TB], F32, "t2") for _ in range(2)]
    yo = [ph.sb([128, 4, TB], BF16, "yo") for _ in range(2)]
    pa = [ph.ps() for _ in range(2)]
    pb = [ph.ps() for _ in range(2)]
    ph.load("pool", wa_t[:], I["w_branch_a"][0].rearrange("(c p) f -> p c f", p=128), "c1", [wa_r])
    ph.load("pool", wb_t[:], I["w_branch_b"][0].rearrange("(c p) f -> p c f", p=128), "c2", [wb_r])
    n = 0
    on = 0
    for tb in range(n_tb):
        tsl = slice(tb * TB, (tb + 1) * TB)
        yat, yar = ya[tb % 2]
        ybt, ybr = yb[tb % 2]
        ph.load("sp", yat[:], T["YAT"][:, :, tsl].rearrange("c p t -> p c t"), f"ya{tb % 2}", [yar])
        ph.load("sp", ybt[:], T["YBT"][:, :, tsl].rearrange("c p t -> p c t"), f"yb{tb % 2}", [ybr])
        for fc in range(16):
            g_t, g_r = gt[n % 3]
            ph.load("sp", g_t[:, 0, :], T["GT"][fc, :, tsl], f"g{n % 3}", [g_r])
            ph.load("sp", g_t[:, 1, :], T["GT"][16 + fc, :, tsl], f"g{n % 3}", [g_r])
            pat, par = pa[n % 2]
            pbt, pbr = pb[n % 2]
            t1t, t1r = t1[n % 2]
            t2t, t2r = t2[n % 2]
            n += 1
            for j in range(4):
                ph.mm(pat[:], wa_t[:, j, fc * 128:(fc + 1) * 128], yat[:, j, :], j == 0, j == 3, [wa_r, yar], [par])
            for j in range(8):
                ph.mm(pbt[:], wb_t[:, j, fc * 128:(fc + 1) * 128], ybt[:, j, :], j == 0, j == 7, [wb_r, ybr], [pbr])
            if fc % 4 == 0:
                yot, yor = yo[on % 2]
                on += 1
            ph.tt("dve", t1t[:], pat[:], g_t[:, 0, :], ALU.mult, [par, g_r], [t1r])
            ph.tt("dve", t2t[:], pbt[:], g_t[:, 1, :], ALU.mult, [pbr, g_r], [t2r])
            ph.tt("pool", yot[:, fc % 4, :], t1t[:], t2t[:], ALU.add, [t1r, t2r], [yor])
            if fc % 4 == 3:
                ph.store("sp", T["YT"][fc - 3:fc + 1, :, tsl].rearrange("c p t -> p c t"), yot[:], f"yo{(on - 1) % 2}", [yor])
    ph.close()


def _layer_norm(ph, h_t, h_r, out_t, out_r, g_t, g_r, b_t, b_r, st, eps_t, eps_r, junk_t, junk_r):
    s_t, s_r = st
    ph.memset("dve", s_t[:], 0.0, [s_r])
    ph.act(junk_t[:], h_t[:], AF.Copy, [h_r, s_r], [junk_r, s_r], accum_out=s_t[:, 0:1])
    ph.act(junk_t[:], h_t[:], AF.Square, [h_r, s_r], [junk_r, s_r], accum_out=s_t[:, 1:2])
    ph.ts("dve", s_t[:, 2:3], s_t[:, 0:1], 1.0 / D, None, ALU.mult, None, [s_r], [s_r])
    ph.tt("dve", s_t[:, 3:4], s_t[:, 2:3], s_t[:, 2:3], ALU.mult, [s_r], [s_r])
    ph.stt("dve", s_t[:, 4:5], s_t[:, 1:2], 1.0 / D, s_t[:, 3:4], ALU.mult, ALU.subtract, [s_r], [s_r])
    ph.act(s_t[:, 5:6], s_t[:, 4:5], AF.Sqrt, [s_r, eps_r], [s_r], bias=eps_t[:, 0:1], scale=1.0)
    ph.sc.op("dve", (lambda h_, a=s_t: h_.reciprocal(a[:, 6:7], a[:, 5:6])), [s_r], [s_r])
    ph.ts("dve", out_t[:], h_t[:], s_t[:, 2:3], s_t[:, 6:7], ALU.subtract, ALU.mult, [h_r, s_r], [out_r])
    ph.tt("pool", out_t[:], out_t[:], g_t[:], ALU.mult, [out_r, g_r], [out_r])
    ph.tt("pool", out_t[:], out_t[:], b_t[:], ALU.add, [out_r, b_r], [out_r])


def phase3b(nc, I, T, n_tb=NTB):
    ph = Phase(nc, "p3b")
    wo_t, wo_r = ph.sb([128, 16, D], BF16, "wo")
    g_t, g_r = ph.sb([128, D], F32, "lng")
    b_t, b_r = ph.sb([128, D], F32, "lnb")
    id_t, id_r = ph.sb([128, 128], BF16, "ident")
    eps_t, eps_r = ph.sb([128, 1], F32, "eps")
    yt = [ph.sb([128, 16, TB], BF16, "yT") for _ in range(2)]
    xc = [ph.sb([128, D], F32, "x") for _ in range(2)]
    h1 = [ph.sb([128, D], F32, "h1") for _ in range(2)]
    x1 = [ph.sb([128, D], F32, "x1") for _ in range(2)]
    x1b = [ph.sb([128, D], BF16, "x1b") for _ in range(2)]
    junk_t, junk_r = ph.sb([128, D], BF16, "junk")
    xT = [ph.sb([128, 16, TB], BF16, "x1T") for _ in range(2)]
    stt_ = [ph.sb([128, 8], F32, "st") for _ in range(2)]
    mixb = [ph.ps() for _ in range(4)]
    trb = [ph.ps([128, 512], BF16, "tr") for _ in range(2)]
    ph.memset("dve", eps_t[:], LN_EPS, [eps_r])
    ph.load("pool", wo_t[:], I["w_out"][0].rearrange("(c p) f -> p c f", p=128), "c1", [wo_r])
    ph.load("sp", g_t[:], I["ln1_g"].partition_broadcast(128), "c2", [g_r])
    ph.load("sp", b_t[:], I["ln1_b"].partition_broadcast(128), "c3", [b_r])
    ph.load("pool", id_t[:], I["c_ident"][:, :], "c4", [id_r])
    n = 0
    trn = 0
    for tb in range(n_tb):
        tsl = slice(tb * TB, (tb + 1) * TB)
        ytt, ytr = yt[tb % 2]
        ph.load("sp", ytt[:], T["YT"][:, :, tsl].rearrange("c p t -> p c t"), f"yt{tb % 2}", [ytr])
        xTt, xTr = xT[tb % 2]
        for tc in range(4):
            r0 = tb * TB + tc * 128
            xct, xcr = xc[n % 2]
            h1t, h1r = h1[n % 2]
            x1t, x1r = x1[n % 2]
            x1bt, x1br = x1b[n % 2]
            st = stt_[n % 2]
            ph.load("sp", xct[:], I["x"][r0:r0 + 128, :], f"x{n % 2}", [xcr])
            for nb in range(4):
                mt, mr = mixb[nb]
                for fc in range(16):
                    ph.mm(mt[:], ytt[:, fc, tc * 128:(tc + 1) * 128], wo_t[:, fc, nb * 512:(nb + 1) * 512], fc == 0, fc == 15, [ytr, wo_r], [mr])
                ph.stt("dve", h1t[:, nb * 512:(nb + 1) * 512], xct[:, nb * 512:(nb + 1) * 512], ALPHA, mt[:], ALU.mult, ALU.add, [xcr, mr], [h1r])
            _layer_norm(ph, h1t, h1r, x1t, x1r, g_t, g_r, b_t, b_r, st, eps_t, eps_r, junk_t, junk_r)
            ph.store("sp", T["X1"][r0:r0 + 128, :], x1t[:], f"x1o{n % 2}", [x1r])
            ph.cp("act", x1bt[:], x1t[:], [x1r], [x1br])
            for q in range(4):
                tt_, ttr = trb[trn % 2]
                trn += 1
                for k in range(4):
                    dc = q * 4 + k
                    ph.tr(tt_[:, k * 128:(k + 1) * 128], x1bt[:, dc * 128:(dc + 1) * 128], id_t[:], [x1br, id_r], [ttr])
                ph.cp("dve" if q % 2 else "act", xTt[:, q * 4:(q + 1) * 4, tc * 128:(tc + 1) * 128],
                      tt_[:].rearrange("p (k t) -> p k t", k=4), [ttr], [xTr])
            n += 1
        ph.store("sp", T["X1T"][:, :, tsl].rearrange("c p t -> p c t"), xTt[:], f"xT{tb % 2}", [xTr])
    ph.close()


def phase4(nc, I, T, n_tb=NTB, n_e=NE):
    ph = Phase(nc, "p4")
    wr_t, wr_r = ph.sb([128, 16, NE], BF16, "wr")
    br_t, br_r = ph.sb([128, NE], F32, "br")
    bg_t, bg_r = ph.sb([128, NE, 16], F32, "bg")
    bl_t, bl_r = ph.sb([128, NE, 16], F32, "bl")
    bd_t, bd_r = ph.sb([NE, D], F32, "bd")
    c_t, c_r = ph.sb([NE, NE * 128], F32, "csel")
    idf_t, idf_r = ph.sb([128, 128], F32, "identf")
    xT_t, xT_r = ph.sb([128, 16, TB], BF16, "x1T")
    hT_t, hT_r = ph.sb([128, 16, TB], BF16, "hT")
    acc_t, acc_r = ph.sb([128, 16, TB], F32, "acc")
    wT_t, wT_r = ph.sb([NE, TB], F32, "WT")
    ring = [ph.sb([128, 16, 512], BF16, "ring") for _ in range(4)]
    lg = [ph.sb([128, NE], F32, "lg") for _ in range(2)]
    t8 = [ph.sb([128, 8], F32, "t8") for _ in range(2)]
    sm = [ph.sb([128, 4], F32, "sm") for _ in range(2)]
    mk = [ph.sb([128, NE], F32, "mk") for _ in range(2)]
    ex = [ph.sb([128, NE], F32, "ex") for _ in range(2)]
    gcl = [ph.sb([128, TB], F32, "gcl") for _ in range(2)]
    sg = [ph.sb([128, TB], F32, "sg") for _ in range(2)]
    l1 = [ph.sb([128, TB], F32, "l1") for _ in range(2)]
    tt1 = [ph.sb([128, TB], F32, "tt1") for _ in range(2)]
    bG = [ph.ps() for _ in range(2)]
    bL = [ph.ps() for _ in range(2)]
    bW = [ph.ps() for _ in range(1)]
    bD = [ph.ps() for _ in range(2)]
    bX = [ph.ps() for _ in range(1)]

    ph.load("pool", wr_t[:], I["w_router"][0].rearrange("(c p) e -> p c e", p=128), "c1", [wr_r])
    ph.load("sp", br_t[:], I["b_router"].partition_broadcast(128), "c2", [br_r])
    ph.load("sp", bg_t[:], I["b_gateT"][:, :, :], "c3", [bg_r])
    ph.load("sp", bl_t[:], I["b_linT"][:, :, :], "c4", [bl_r])
    ph.ts("dve", bl_t[:], bl_t[:], 1.0, None, ALU.add, None, [bl_r], [bl_r])
    ph.load("sp", bd_t[:], I["b_down"][0, :, :], "c5", [bd_r])
    ph.load("sp", c_t[:], I["c_csel"][:, :], "c6", [c_r])
    ph.load("sp", idf_t[:], I["c_ident"][:, :], "c7", [idf_r])

    rn = [0]

    def wload(src):
        slot = rn[0] % 4
        rn[0] += 1
        t, r = ring[slot]
        ph.load("pool", t[:], src.rearrange("(c p) f -> p c f", p=128), f"r{slot}", [r])
        return t, r

    k = 0
    dn = 0
    for tb in range(n_tb):
        tsl = slice(tb * TB, (tb + 1) * TB)
        ph.load("sp", xT_t[:], T["X1TS"][:, :, tsl].rearrange("c p t -> p c t"), "x1T", [xT_r])
        for tc in range(4):
            lgt, lgr = lg[tc % 2]
            t8t, t8r = t8[tc % 2]
            smt, smr = sm[tc % 2]
            mkt, mkr = mk[tc % 2]
            ext, exr = ex[tc % 2]
            xb, xbr = bX[0]
            for dc in range(16):
                ph.mm(xb[:, 0:NE], xT_t[:, dc, tc * 128:(tc + 1) * 128], wr_t[:, dc, :], dc == 0, dc == 15, [xT_r, wr_r], [xbr])
            ph.tt("dve", lgt[:], xb[:, 0:NE], br_t[:], ALU.add, [xbr, br_r], [lgr])
            ph.sc.op("dve", (lambda h_, a=t8t, b_=lgt: h_.max(a[:], b_[:])), [lgr], [t8r])
            ph.ts("dve", mkt[:], lgt[:], t8t[:, 3:4], None, ALU.is_ge, None, [lgr, t8r], [mkr])
            ph.ts("dve", smt[:, 0:1], t8t[:, 0:1], -1.0, None, ALU.mult, None, [t8r], [smr])
            ph.act(ext[:], lgt[:], AF.Exp, [lgr, smr], [exr], bias=smt[:, 0:1])
            ph.tt("dve", ext[:], ext[:], mkt[:], ALU.mult, [exr, mkr], [exr])
            ph.sc.op("dve", (lambda h_, a=smt, b_=ext: h_.reduce_sum(a[:, 1:2], b_[:], mybir.AxisListType.X)), [exr], [smr])
            ph.sc.op("dve", (lambda h_, a=smt: h_.reciprocal(a[:, 2:3], a[:, 1:2])), [smr], [smr])
            ph.ts("dve", ext[:], ext[:], smt[:, 2:3], None, ALU.mult, None, [exr, smr], [exr])
            ph.tr(xb[0:NE, 128:256], ext[:], idf_t[:], [exr, idf_r], [xbr])
            ph.cp("dve", wT_t[:, tc * 128:(tc + 1) * 128], xb[0:NE, 128:256], [xbr], [wT_r])
        if "WT" in T:
            ph.store("sp", T["WT"][:, tsl], wT_t[:], "wto", [wT_r])
        for nch in range(16):
            dt_, dr = bD[dn % 2]
            dn += 1
            ph.mm(dt_[:], bd_t[:, nch * 128:(nch + 1) * 128], wT_t[:], True, True, [bd_r, wT_r], [dr])
            ph.cp("act", acc_t[:, nch, :], dt_[:], [dr], [acc_r])
        for e in range(n_e):
            wb, wbr = bW[0]
            ph.mm(wb[:], c_t[:, e * 128:(e + 1) * 128], wT_t[:], True, True, [c_r, wT_r], [wbr])
            for fb in range(4):
                fsl = slice(fb * 512, (fb + 1) * 512)
                wg, wgr = wload(I["w_gate"][0, e, :, fsl])
                wl, wlr = wload(I["w_lin"][0, e, :, fsl])
                for fcl in range(4):
                    fc = fb * 4 + fcl
                    gt_, gr_ = bG[k % 2]
                    lt_, lr_ = bL[k % 2]
                    gc, gcr = gcl[k % 2]
                    sgt, sgr = sg[k % 2]
                    l1t, l1r = l1[k % 2]
                    t1t, t1r = tt1[k % 2]
                    k += 1
                    for dc in range(16):
                        ph.mm(gt_[:], wg[:, dc, fcl * 128:(fcl + 1) * 128], xT_t[:, dc, :], dc == 0, dc == 15, [wgr, xT_r], [gr_])
                    for dc in range(16):
                        ph.mm(lt_[:], wl[:, dc, fcl * 128:(fcl + 1) * 128], xT_t[:, dc, :], dc == 0, dc == 15, [wlr, xT_r], [lr_])
                    ph.ts("dve", gc[:], gt_[:], bg_t[:, e, fc:fc + 1], 7.0, ALU.add, ALU.min, [gr_, bg_r], [gcr])
                    ph.act(sgt[:], gc[:], AF.Sigmoid, [gcr], [sgr], scale=1.702)
                    ph.ts("dve", l1t[:], lt_[:], bl_t[:, e, fc:fc + 1], 8.0, ALU.add, ALU.min, [lr_, bl_r], [l1r])
                    ph.tt("dve", t1t[:], gc[:], wb[:], ALU.mult, [gcr, wbr], [t1r])
                    ph.stt("dve", t1t[:], l1t[:], -6.0, t1t[:], ALU.max, ALU.mult, [l1r, t1r], [t1r])
                    ph.tt("dve", hT_t[:, fc, :], t1t[:], sgt[:], ALU.mult, [t1r, sgr], [hT_r])
            for nb in range(4):
                wd, wdr = wload(I["w_down"][0, e, :, nb * 512:(nb + 1) * 512])
                for ncl in range(4):
                    nch = nb * 4 + ncl
                    dt_, dr = bD[dn % 2]
                    dn += 1
                    for fc in range(16):
                        ph.mm(dt_[:], wd[:, fc, ncl * 128:(ncl + 1) * 128], hT_t[:, fc, :], fc == 0, fc == 15, [wdr, hT_r], [dr])
                    ph.tt("dve", acc_t[:, nch, :], acc_t[:, nch, :], dt_[:], ALU.add, [acc_r, dr], [acc_r])
        ph.store("sp", T["FT"][:, :, tsl].rearrange("c p t -> p c t"), acc_t[:], "fto", [acc_r])
    ph.close()


def phase5(nc, I, T, out_ap, n_tb=NTB):
    ph = Phase(nc, "p5")
    g_t, g_r = ph.sb([128, D], F32, "lng")
    b_t, b_r = ph.sb([128, D], F32, "lnb")
    idf_t, idf_r = ph.sb([128, 128], F32, "identf")
    eps_t, eps_r = ph.sb([128, 1], F32, "eps")
    ft = [ph.sb([128, 16, 128], F32, "ft") for _ in range(2)]
    xc = [ph.sb([128, D], F32, "x1") for _ in range(2)]
    h2 = [ph.sb([128, D], F32, "h2") for _ in range(2)]
    ob = [ph.sb([128, D], F32, "ob") for _ in range(2)]
    junk_t, junk_r = ph.sb([128, D], BF16, "junk")
    stt_ = [ph.sb([128, 8], F32, "st") for _ in range(2)]
    banks = [ph.ps() for _ in range(8)]
    ph.memset("dve", eps_t[:], LN_EPS, [eps_r])
    ph.load("sp", g_t[:], I["ln2_g"].partition_broadcast(128), "c1", [g_r])
    ph.load("sp", b_t[:], I["ln2_b"].partition_broadcast(128), "c2", [b_r])
    ph.load("sp", idf_t[:], I["c_ident"][:, :], "c3", [idf_r])
    for n in range(n_tb * 4):
        r0 = n * 128
        ftt, ftr = ft[n % 2]
        xct, xcr = xc[n % 2]
        h2t, h2r = h2[n % 2]
        obt, obr = ob[n % 2]
        ph.load("sp", ftt[:], T["FT"][:, :, r0:r0 + 128].rearrange("c p t -> p c t"), f"ft{n % 2}", [ftr])
        ph.load("sp", xct[:], T["X1S"][r0:r0 + 128, :], f"x{n % 2}", [xcr])
        for q in range(4):
            bt, br = banks[(n % 2) * 4 + q]
            for kq in range(4):
                nch = q * 4 + kq
                ph.tr(bt[:, kq * 128:(kq + 1) * 128], ftt[:, nch, :], idf_t[:], [ftr, idf_r], [br])
            ph.stt("dve", h2t[:, q * 512:(q + 1) * 512], xct[:, q * 512:(q + 1) * 512], ALPHA, bt[:], ALU.mult, ALU.add, [xcr, br], [h2r])
        _layer_norm(ph, h2t, h2r, obt, obr, g_t, g_r, b_t, b_r, stt_[n % 2], eps_t, eps_r, junk_t, junk_r)
        ph.store("sp", out_ap[r0:r0 + 128, :], obt[:], f"o{n % 2}", [obr])
    ph.close()


def phaseS(nc, I, T, n_half, n_sel):
    ph = Phase(nc, "pS")
    ntc = n_half * 4
    sel_t, sel_r = ph.sb([128, ntc, 128], F32, "sel")
    id_t, id_r = ph.sb([128, 128], BF16, "ident")
    xc = [ph.sb([128, D], F32, "x") for _ in range(3)]
    xs = [ph.sb([128, D], F32, "xs") for _ in range(2)]
    xb = [ph.sb([128, D], BF16, "xb") for _ in range(2)]
    xT = [ph.sb([128, 16, TB], BF16, "x1T") for _ in range(2)]
    banks = [ph.ps() for _ in range(4)]
    trb = [ph.ps([128, 512], BF16, "tr") for _ in range(2)]
    ph.load("pool", id_t[:], I["c_ident"][:, :], "c", [id_r])
    xn = 0
    trn = 0
    for jc in range(n_sel * 4):
        ph.load("sp", sel_t[:], I["sel"][:, jc * 128:(jc + 1) * 128].rearrange("(c p) j -> p c j", p=128), "sel", [sel_r])
        xst, xsr = xs[jc % 2]
        xbt, xbr = xb[jc % 2]
        xTt, xTr = xT[(jc // 4) % 2]
        for tcn in range(ntc):
            xct, xcr = xc[xn % 3]
            ph.load("sp", xct[:], T["X1"][tcn * 128:(tcn + 1) * 128, :], f"x{xn % 3}", [xcr])
            xn += 1
            for nb in range(4):
                bt, br = banks[nb]
                ph.mm(bt[:], sel_t[:, tcn, :], xct[:, nb * 512:(nb + 1) * 512], tcn == 0, tcn == ntc - 1, [sel_r, xcr], [br])
        for nb in range(4):
            bt, br = banks[nb]
            ph.cp("dve" if nb % 2 else "act", xst[:, nb * 512:(nb + 1) * 512], bt[:], [br], [xsr])
        ph.store("sp", T["X1S"][jc * 128:(jc + 1) * 128, :], xst[:], f"xs{jc % 2}", [xsr])
        ph.cp("act", xbt[:], xst[:], [xsr], [xbr])
        tc = jc % 4
        for q in range(4):
            tt_, ttr = trb[trn % 2]
            trn += 1
            for k in range(4):
                dc = q * 4 + k
                ph.tr(tt_[:, k * 128:(k + 1) * 128], xbt[:, dc * 128:(dc + 1) * 128], id_t[:], [xbr, id_r], [ttr])
            ph.cp("dve" if q % 2 else "act", xTt[:, q * 4:(q + 1) * 4, tc * 128:(tc + 1) * 128],
                  tt_[:].rearrange("p (k t) -> p k t", k=4), [ttr], [xTr])
        if tc == 3:
            tb = jc // 4
            ph.store("sp", T["X1TS"][:, :, tb * TB:(tb + 1) * TB].rearrange("c p t -> p c t"), xTt[:], f"xT{tb % 2}", [xTr])
    ph.close()


ALL_PHASES = ("0", "1", "2a", "2b", "3a", "3b", "S", "4", "5")


N_DIR = 2
N_PER = 4
N_CORES = N_DIR * N_PER


def kernel(**inputs):
    half_tb = NTB // N_DIR
    sel_tb = half_tb // N_PER
    nc = build(phases=ALL_PHASES, n_tb=half_tb, n_sel=sel_tb)
    maps = []
    for c in range(N_CORES):
        maps.append(host_inputs(inputs, rev=(c // N_PER == 1), n_half=half_tb, n_sel=sel_tb, q=c % N_PER))
    res = run_bass_kernel_spmd(nc, maps, core_ids=list(range(N_CORES)))
    parts = []
    for c in range(N_PER):
        parts.append(np.asarray(res.results[c]["out"], dtype=np.float32))
    for c in reversed(range(N_PER)):
        parts.append(np.asarray(res.results[N_PER + c]["out"], dtype=np.float32)[::-1])
    out = np.concatenate(parts, axis=0)
    return out.reshape(1, S, D)
```
